# Optimizing a Trainium2 kernel written in Bass

```python
import math
import jax
import jax.numpy as jnp
from jax import lax
import numpy as np

D_MODEL = 1024
BATCH = 4
SEQ = 4096
DEPTH = 1

MLA_HEADS = 8
MLA_NOPE = 128
MLA_ROPE = 64
MLA_V = 128
Q_LORA = 384
KV_LORA = 256
ROPE_BASE = 10000.0
ATTN_BLOCK = 128
RET_HEADS = 8
RET_DK = 128
RET_DV = 128
RET_CHUNK = 128
N_EXPERTS = 32
TOP_K = 4
D_EXPERT = 1024
SWIGLU_LIMIT = 7.0
SWIGLU_ALPHA = 1.702
EXPERT_BLOCK = 128
MAX_POS_OFFSET = 2048
DN_ALPHA = (2.0 * DEPTH) ** 0.25
DN_BETA = (8.0 * DEPTH) ** -0.25
LN_EPS = 1e-5
RMS_EPS = 1e-6
GN_EPS = 1e-6
NEG_INF = -1e30
IN_SPLITS = (Q_LORA, KV_LORA, MLA_ROPE,
             RET_HEADS * RET_DK, RET_HEADS * RET_DK,
             RET_HEADS * RET_DV, RET_HEADS * RET_DV,
             D_MODEL, D_MODEL)
D_IN = Q_LORA + KV_LORA + MLA_ROPE + 2 * RET_HEADS * RET_DK + 2 * RET_HEADS * RET_DV + 2 * D_MODEL

kernel_name = 'hybrid_mla_retention_moe_deepnorm'


def _layernorm(x, g, b):
    xf = x.astype(jnp.float32)
    mu = jnp.mean(xf, axis=-1, keepdims=True)
    var = jnp.mean(jnp.square(xf - mu), axis=-1, keepdims=True)
    return ((xf - mu) * lax.rsqrt(var + LN_EPS) * g + b).astype(x.dtype)


def _rmsnorm(x, g):
    xf = x.astype(jnp.float32)
    return (xf * lax.rsqrt(jnp.mean(jnp.square(xf), axis=-1, keepdims=True) + RMS_EPS) * g).astype(x.dtype)


def _rotary(x, positions, inv_freq):
    ang = positions.astype(jnp.float32)[:, :, None] * inv_freq[None, None, :]
    cos = jnp.cos(ang)[:, :, None, :]
    sin = jnp.sin(ang)[:, :, None, :]
    xf = x.astype(jnp.float32)
    half = x.shape[-1] // 2
    x1, x2 = xf[..., :half], xf[..., half:]
    return jnp.concatenate([x1 * cos - x2 * sin, x2 * cos + x1 * sin], axis=-1).astype(x.dtype)


def _causal_block_attention(q, k, v):
    B, S, H, Dq = q.shape
    nq = S // ATTN_BLOCK
    scale = Dq ** -0.5
    qb = q.reshape(B, nq, ATTN_BLOCK, H, Dq).transpose(1, 0, 2, 3, 4)
    starts = jnp.arange(nq, dtype=jnp.int32) * ATTN_BLOCK
    key_pos = jnp.arange(S, dtype=jnp.int32)

    def one_block(args):
        q_blk, start = args
        s = jnp.einsum('bqhd,bkhd->bhqk', q_blk, k).astype(jnp.float32) * scale
        q_pos = start + jnp.arange(ATTN_BLOCK, dtype=jnp.int32)
        causal = (q_pos[:, None] >= key_pos[None, :])[None, None]
        p = jax.nn.softmax(jnp.where(causal, s, NEG_INF), axis=-1).astype(v.dtype)
        return jnp.einsum('bhqk,bkhd->bqhd', p, v)

    o = lax.map(one_block, (qb, starts))
    return o.transpose(1, 0, 2, 3, 4).reshape(B, S, H, v.shape[-1])


def _retention(q, k, v, positions):
    B, S, H, Dk = q.shape
    Dv = v.shape[-1]
    C = RET_CHUNK
    N = S // C
    inv_freq = 1.0 / (10000.0 ** jnp.linspace(0.0, 1.0, Dk // 2, dtype=jnp.float32))
    q = _rotary(q, positions, inv_freq)
    k = _rotary(k, positions, inv_freq) * (Dk ** -0.5)
    log_gamma = jnp.log(1.0 - 2.0 ** (-5.0 - jnp.arange(H, dtype=jnp.float32)))
    idx = jnp.arange(C, dtype=jnp.float32)
    rel = idx[:, None] - idx[None, :]
    decay = jnp.where(rel[None] >= 0,
                      jnp.exp(jnp.maximum(rel, 0.0)[None] * log_gamma[:, None, None]), 0.0)
    qc = q.reshape(B, N, C, H, Dk)
    kc = k.reshape(B, N, C, H, Dk)
    vc = v.reshape(B, N, C, H, Dv)
    scores = jnp.einsum('bnihd,bnjhd->bnhij', qc, kc).astype(jnp.float32) * decay[None, None]
    o_inner = jnp.einsum('bnhij,bnjhe->bnihe', scores, vc.astype(jnp.float32))
    k_w = jnp.exp((C - 1.0 - idx)[:, None] * log_gamma[None, :])
    q_w = jnp.exp((idx + 1.0)[:, None] * log_gamma[None, :])
    U = jnp.einsum('bnjhd,bnjhe->nbhde', kc.astype(jnp.float32) * k_w[:, :, None],
                   vc.astype(jnp.float32))
    chunk_decay = jnp.exp(C * log_gamma)[None, :, None, None]

    def step(R, U_n):
        return chunk_decay * R + U_n, R

    _, R_prev = lax.scan(step, jnp.zeros((B, H, Dk, Dv), jnp.float32), U)
    o_cross = jnp.einsum('bnihd,nbhde->bnihe', qc.astype(jnp.float32) * q_w[:, :, None], R_prev)
    o = (o_inner + o_cross).reshape(B, S, H, Dv)
    mu = jnp.mean(o, axis=-1, keepdims=True)
    var = jnp.mean(jnp.square(o - mu), axis=-1, keepdims=True)
    return ((o - mu) * lax.rsqrt(var + GN_EPS)).astype(v.dtype)


def _hybrid_mixer(x, positions, w_in, q_norm_g, w_uq, kv_norm_g, w_ukv, w_o):
    B, S, _ = x.shape
    split_at = [int(i) for i in np.cumsum(IN_SPLITS)[:-1]]
    proj = x @ w_in
    c_q, c_kv, k_r, r_q, r_k, r_v, r_g, g_mla, g_ret = jnp.split(proj, split_at, axis=-1)
    inv_freq = 1.0 / (ROPE_BASE ** (jnp.arange(0, MLA_ROPE, 2, dtype=jnp.float32) / MLA_ROPE))
    q = (_rmsnorm(c_q, q_norm_g) @ w_uq).reshape(B, S, MLA_HEADS, MLA_NOPE + MLA_ROPE)
    q = jnp.concatenate([q[..., :MLA_NOPE], _rotary(q[..., MLA_NOPE:], positions, inv_freq)], axis=-1)
    kv = (_rmsnorm(c_kv, kv_norm_g) @ w_ukv).reshape(B, S, MLA_HEADS, MLA_NOPE + MLA_V)
    k_nope, v = kv[..., :MLA_NOPE], kv[..., MLA_NOPE:]
    k_rope = _rotary(k_r[:, :, None, :], positions, inv_freq)
    k = jnp.concatenate([k_nope, jnp.broadcast_to(k_rope, (B, S, MLA_HEADS, MLA_ROPE))], axis=-1)
    o_mla = _causal_block_attention(q, k, v).reshape(B, S, MLA_HEADS * MLA_V)
    o_ret = _retention(r_q.reshape(B, S, RET_HEADS, RET_DK), r_k.reshape(B, S, RET_HEADS, RET_DK),
                       r_v.reshape(B, S, RET_HEADS, RET_DV), positions)
    o_ret = o_ret.reshape(B, S, RET_HEADS * RET_DV) * jax.nn.silu(r_g)
    mixed = jax.nn.sigmoid(g_mla) * o_mla + jax.nn.sigmoid(g_ret) * o_ret
    return (mixed @ w_o).astype(x.dtype)


def _routed_experts(h, w_router, b_router, w_up, b_up, w_down, b_down):
    Bn, Sn, D = h.shape
    T = Bn * Sn
    xt = h.reshape(T, D)
    logits = (xt @ w_router + b_router).astype(jnp.float32)
    top_v, top_i = lax.top_k(logits, TOP_K)
    gate = jax.nn.softmax(top_v, axis=-1)
    n_assign = T * TOP_K
    flat_e = top_i.reshape(-1).astype(jnp.int32)
    flat_tok = jnp.arange(n_assign, dtype=jnp.int32) // TOP_K
    flat_w = gate.reshape(-1)
    order = jnp.argsort(flat_e)
    se = flat_e[order]
    counts = jnp.bincount(flat_e, length=N_EXPERTS).astype(jnp.int32)
    padded = ((counts + EXPERT_BLOCK - 1) // EXPERT_BLOCK) * EXPERT_BLOCK
    pad_end = jnp.cumsum(padded)
    pad_start = pad_end - padded
    grp_start = jnp.cumsum(counts) - counts
    rank = jnp.arange(n_assign, dtype=jnp.int32) - grp_start[se]
    dest = pad_start[se] + rank
    P = n_assign + N_EXPERTS * EXPERT_BLOCK
    n_blocks = P // EXPERT_BLOCK
    row_tok = jnp.zeros((P,), jnp.int32).at[dest].set(flat_tok[order])
    row_w = jnp.zeros((P,), jnp.float32).at[dest].set(flat_w[order])
    row_valid = jnp.zeros((P,), bool).at[dest].set(True)
    blk_e = jnp.minimum(jnp.searchsorted(pad_end, jnp.arange(n_blocks, dtype=jnp.int32) * EXPERT_BLOCK,
                                         side='right'), N_EXPERTS - 1).astype(jnp.int32)
    xs = xt[row_tok].reshape(n_blocks, EXPERT_BLOCK, D)

    def expert_block(args):
        xb, e = args
        hcat = xb @ w_up[e] + b_up[e]
        h_glu = jnp.minimum(hcat[:, :D_EXPERT], SWIGLU_LIMIT)
        h_lin = jnp.clip(hcat[:, D_EXPERT:], -SWIGLU_LIMIT, SWIGLU_LIMIT)
        a = h_glu * jax.nn.sigmoid(SWIGLU_ALPHA * h_glu) * (h_lin + 1.0)
        return a @ w_down[e] + b_down[e]

    ys = lax.map(expert_block, (xs, blk_e)).reshape(P, D)
    ys = jnp.where(row_valid[:, None], ys * row_w[:, None], 0.0)
    out = jax.ops.segment_sum(ys, row_tok, num_segments=T)
    return out.reshape(Bn, Sn, D).astype(h.dtype)


def setup_inputs(seed: int = 0) -> dict:
    key = jax.random.key(seed)
    ks = jax.random.split(key, 20)
    f32 = jnp.float32

    def nrm(k, shape, scale):
        return jax.random.normal(k, shape, f32) * scale

    x = nrm(ks[0], (BATCH, SEQ, D_MODEL), 1.0)
    offset = jax.random.randint(ks[1], (BATCH, 1), 0, MAX_POS_OFFSET, dtype=jnp.int32)
    positions = offset + jnp.arange(SEQ, dtype=jnp.int32)[None, :]
    return {
        'x': x,
        'positions': positions,
        'w_in': nrm(ks[2], (DEPTH, D_MODEL, D_IN), D_MODEL ** -0.5),
        'q_norm_g': 1.0 + nrm(ks[3], (DEPTH, Q_LORA), 0.02),
        'w_uq': nrm(ks[4], (DEPTH, Q_LORA, MLA_HEADS * (MLA_NOPE + MLA_ROPE)), Q_LORA ** -0.5),
        'kv_norm_g': 1.0 + nrm(ks[5], (DEPTH, KV_LORA), 0.02),
        'w_ukv': nrm(ks[6], (DEPTH, KV_LORA, MLA_HEADS * (MLA_NOPE + MLA_V)), KV_LORA ** -0.5),
        'w_o': nrm(ks[7], (DEPTH, D_MODEL, D_MODEL), D_MODEL ** -0.5 * DN_BETA),
        'ln1_g': 1.0 + nrm(ks[8], (DEPTH, D_MODEL), 0.02),
        'ln1_b': nrm(ks[9], (DEPTH, D_MODEL), 0.02),
        'w_router': nrm(ks[10], (DEPTH, D_MODEL, N_EXPERTS), D_MODEL ** -0.5),
        'b_router': nrm(ks[11], (DEPTH, N_EXPERTS), 0.01),
        'w_up': nrm(ks[12], (DEPTH, N_EXPERTS, D_MODEL, 2 * D_EXPERT), D_MODEL ** -0.5),
        'b_up': nrm(ks[13], (DEPTH, N_EXPERTS, 2 * D_EXPERT), 0.01),
        'w_down': nrm(ks[14], (DEPTH, N_EXPERTS, D_EXPERT, D_MODEL), D_EXPERT ** -0.5 * DN_BETA),
        'b_down': nrm(ks[15], (DEPTH, N_EXPERTS, D_MODEL), 0.01),
        'ln2_g': 1.0 + nrm(ks[16], (DEPTH, D_MODEL), 0.02),
        'ln2_b': nrm(ks[17], (DEPTH, D_MODEL), 0.02),
    }


def reference(x, positions, w_in, q_norm_g, w_uq, kv_norm_g, w_ukv, w_o, ln1_g, ln1_b,
              w_router, b_router, w_up, b_up, w_down, b_down, ln2_g, ln2_b):
    h = x
    for l in range(DEPTH):
        mix = _hybrid_mixer(h, positions, w_in[l], q_norm_g[l], w_uq[l], kv_norm_g[l], w_ukv[l], w_o[l])
        h = _layernorm(DN_ALPHA * h + mix, ln1_g[l], ln1_b[l])
        ffn = _routed_experts(h, w_router[l], b_router[l], w_up[l], b_up[l], w_down[l], b_down[l])
        h = _layernorm(DN_ALPHA * h + ffn, ln2_g[l], ln2_b[l])
    return h
```

```python
import numpy as np
import concourse.bass as bass
import concourse.mybir as mybir
from concourse.bass_utils import run_bass_kernel_spmd
from contextlib import ExitStack

F32 = mybir.dt.float32
BF16 = mybir.dt.bfloat16
I32 = mybir.dt.int32
ALU = mybir.AluOpType
AF = mybir.ActivationFunctionType
AX = mybir.AxisListType

ENGS = ["pe", "act", "dve", "pool", "sp"]
NPAIR = 16
DN_ALPHA = 2.0 ** 0.25
SM_SCALE = 192.0 ** -0.5


class Buf:
    def __init__(self, name):
        self.name = name
        self.w = {}
        self.r = {}


class Sched:
    def __init__(self, nc):
        self.nc = nc
        self.ops = {e: [] for e in ENGS}
        self.events = {}
        self.known = {e: {} for e in ENGS}
        self.pending = {e: [] for e in ENGS}

    def add(self, eng, fn, reads=(), writes=(), dsem=None):
        semkey = dsem if dsem is not None else eng
        rec = {"eng": eng, "fn": fn, "waits": [], "inc": dsem is not None, "semkey": semkey,
               "is_dma": dsem is not None}
        evs = self.events.setdefault(semkey, [])
        evs.append(rec)
        rec["ord"] = len(evs)
        need = {}
        for b in reads:
            for k, (o, r) in b.w.items():
                if k not in need or need[k][0] < o:
                    need[k] = (o, r)
        for b in writes:
            for d in (b.w, b.r):
                for k, (o, r) in d.items():
                    if k not in need or need[k][0] < o:
                        need[k] = (o, r)
        for (k, o, r) in self.pending[eng]:
            if k not in need or need[k][0] < o:
                need[k] = (o, r)
        self.pending[eng] = []
        kn = self.known[eng]
        for k, (o, r) in need.items():
            if r is rec:
                continue
            if k == eng and not rec["is_dma"] and eng == "pe":
                continue
            if kn.get(k, 0) >= o:
                continue
            kn[k] = o
            r["inc"] = True
            rec["waits"].append(r)
        for b in reads:
            b.r[semkey] = (rec["ord"], rec)
        for b in writes:
            b.w[semkey] = (rec["ord"], rec)
        self.ops[eng].append(rec)
        return rec

    def barrier(self):
        last = []
        for k, evs in self.events.items():
            if evs:
                last.append((k, evs[-1]["ord"], evs[-1]))
        for e in ENGS:
            self.pending[e] = list(last)

    def emit(self, es, final_dma_sems=()):
        nc = self.nc
        sems = {}
        for k in self.events:
            sems[k] = es.enter_context(nc.semaphore("s_" + k))
        for k, evs in self.events.items():
            c = 0
            for r in evs:
                if r["is_dma"]:
                    c += 16
                    r["val"] = c
                elif r["inc"]:
                    c += 1
                    r["val"] = c
        ops = self.ops
        events = self.events

        def run(engname, eh):
            for r in ops[engname]:
                for w in r["waits"]:
                    eh.wait_ge(sems[w["semkey"]], w["val"])
                ins = r["fn"](eh)
                if r["is_dma"]:
                    ins.then_inc(sems[r["semkey"]], 16)
                elif r["inc"]:
                    ins.then_inc(sems[r["semkey"]], 1)
            if engname == "sp":
                for k in final_dma_sems:
                    evs = events.get(k)
                    if evs:
                        eh.wait_ge(sems[k], evs[-1]["val"])

        with nc.Block() as block:
            @block.tensor
            def _(e):
                run("pe", e)

            @block.scalar
            def _(e):
                run("act", e)

            @block.vector
            def _(e):
                run("dve", e)

            @block.gpsimd
            def _(e):
                run("pool", e)

            @block.sync
            def _(e):
                run("sp", e)


def build_program(debug=False):
    nc = bass.Bass("TRN2", target_bir_lowering=False)

    def din(name, shape, dt=F32):
        return nc.dram_tensor(name, list(shape), dt, kind="ExternalInput").ap()

    xT_all = din("xT_all", [128, 8, 4096])
    xT_own = din("xT_own", [128, 8, 2048])
    x_own = din("x_own", [16, 128, 1024])
    pos_all = din("pos_all", [128, 32], I32)
    pos_own = din("pos_own", [128, 16], I32)
    d_wcq = din("wcq", [128, 8, 384])
    d_wckv = din("wckv", [128, 8, 320])
    d_wrq = din("wrq", [128, 8, 1024])
    d_wrk = din("wrk", [128, 8, 1024])
    d_wrv = din("wrv", [128, 8, 1024])
    d_wgate = din("wgate", [8, 128, 8, 384])
    d_wuq = din("wuq", [128, 3, 1536])
    d_qg = din("qg", [128, 3])
    d_wukv = din("wukv", [128, 2, 2048])
    d_kvg = din("kvg", [128, 2])
    d_wo = din("wo", [128, 8, 1024])
    d_ln = din("lnp", [4, 1024])
    d_wr = din("wr", [128, 8, 32])
    d_br = din("br", [1, 32])
    d_wup = din("wup", [32, 4, 128, 8, 512])
    d_bup = din("bup", [128, 32, 16])
    d_wdn = din("wdn", [32, 2, 128, 4, 1024])
    d_bdn = din("bdn", [32, 1024])
    d_tab = din("tab", [128, 2, 8, 128])
    d_qw = din("qwt", [128, 8, 128])
    d_kw = din("kwt", [128, 2, 8])
    d_cd2 = din("cd2", [128, 8])
    d_mask = din("mask", [128, 2, 128])
    d_invf = din("invf", [128, 96])
    out_d = nc.dram_tensor("out", [16, 128, 1024], F32, kind="ExternalOutput").ap()

    with ExitStack() as es:
        ARENA_BYTES = 207 * 1024
        AR = es.enter_context(nc.sbuf_tensor("arena", [128, ARENA_BYTES // 4], F32))
        PS = [es.enter_context(nc.psum_tensor("psb%d" % i, [128, 512], F32)) for i in range(8)]
        PB = [Buf("ps%d" % i) for i in range(8)]
        S = Sched(nc)
        top = [0]
        uid = [0]

        class T:
            pass

        def alloc(shape, dt, name=None):
            esz = 4 if dt in (F32, I32) else 2
            n = int(np.prod(shape[1:])) * esz
            n = (n + 31) // 32 * 32
            off = top[0]
            top[0] += n
            assert top[0] <= ARENA_BYTES, ("arena overflow", name, top[0])
            v = AR[:shape[0], off // 4:(off + n) // 4]
            nelem = int(np.prod(shape[1:]))
            if dt == BF16:
                v = v.bitcast(BF16)[:, 0:nelem]
            elif dt == I32:
                v = v.bitcast(I32)[:, 0:nelem]
            else:
                v = v[:, 0:nelem]
            if len(shape) == 3:
                v = v.rearrange("p (a b) -> p a b", a=shape[1])
            elif len(shape) == 4:
                v = v.rearrange("p (a b c) -> p a b c", a=shape[1], b=shape[2])
            t = T()
            t.ap = v
            uid[0] += 1
            t.b = Buf((name or "t") + str(uid[0]))
            t.name = t.b.name
            return t

        def MM(out, lhsT, rhs, start, stop, R, W):
            S.add("pe", lambda e: e.matmul(out, lhsT=lhsT, rhs=rhs, start=start, stop=stop), R, W)

        def TR(out, in_, ident, R, W):
            S.add("pe", lambda e: e.transpose(out, in_, ident), R, W)

        def ACT(out, in_, func, R, W, bias=0.0, scale=1.0, accum=None):
            if accum is None:
                S.add("act", lambda e: e.activation(out=out, in_=in_, func=func, bias=bias, scale=scale), R, W)
            else:
                S.add("act", lambda e: e.activation(out=out, in_=in_, func=func, bias=bias, scale=scale, accum_out=accum), R, W)

        def TT(eng, out, in0, in1, op, R, W):
            S.add(eng, lambda e: e.tensor_tensor(out=out, in0=in0, in1=in1, op=op), R, W)

        def TS(eng, out, in0, s1, s2, op0, op1, R, W):
            if s2 is None:
                S.add(eng, lambda e: e.tensor_scalar(out=out, in0=in0, scalar1=s1, scalar2=None, op0=op0), R, W)
            else:
                S.add(eng, lambda e: e.tensor_scalar(out=out, in0=in0, scalar1=s1, scalar2=s2, op0=op0, op1=op1), R, W)

        def STT(eng, out, in0, scalar, in1, op0, op1, R, W):
            S.add(eng, lambda e: e.scalar_tensor_tensor(out=out, in0=in0, scalar=scalar, in1=in1, op0=op0, op1=op1), R, W)

        def CP(eng, out, in_, R, W):
            if eng == "act":
                S.add("act", lambda e: e.copy(out=out, in_=in_), R, W)
            else:
                S.add(eng, lambda e: e.tensor_copy(out=out, in_=in_), R, W)

        def DMA(eng, out, in_, R, W, dsem):
            S.add(eng, lambda e: e.dma_start(out=out, in_=in_), R, W, dsem=dsem)

        def bc(ap, shape, axis):
            return ap.unsqueeze(axis).to_broadcast(list(shape))

        ident_f = alloc([128, 128], F32, "identf")
        ident_b = alloc([128, 128], BF16, "identb")
        ones_b = alloc([128, 128], BF16, "ones")
        S.add("pool", lambda e: e.iota(ident_f.ap, [[1, 128]], base=0, channel_multiplier=-1,
                                       allow_small_or_imprecise_dtypes=True), (), [ident_f.b])
        S.add("dve", lambda e: e.tensor_single_scalar(out=ident_f.ap, in_=ident_f.ap, scalar=0.0, op=ALU.is_equal),
              [ident_f.b], [ident_f.b])
        CP("dve", ident_b.ap, ident_f.ap, [ident_f.b], [ident_b.b])
        S.add("pool", lambda e: e.memset(ones_b.ap, 1.0), (), [ones_b.b])
        mask = alloc([128, 2, 128], F32, "mask")
        DMA("sp", mask.ap, d_mask, (), [mask.b], "d_c0")
        posA_i = alloc([128, 32], I32, "posAi")
        posO_i = alloc([128, 16], I32, "posOi")
        posA = alloc([128, 32], F32, "posA")
        posO = alloc([128, 16], F32, "posO")
        invf = alloc([128, 96], F32, "invf")
        DMA("sp", posA_i.ap, pos_all, (), [posA_i.b], "d_c1")
        DMA("sp", posO_i.ap, pos_own, (), [posO_i.b], "d_c2")
        DMA("sp", invf.ap, d_invf, (), [invf.b], "d_c3")
        CP("dve", posA.ap, posA_i.ap, [posA_i.b], [posA.b])
        CP("dve", posO.ap, posO_i.ap, [posO_i.b], [posO.b])
        latent_off = top[0]
        cqnT = alloc([128, 3, 2048], BF16, "cqnT")
        ckvnT = alloc([128, 2, 4096], BF16, "ckvnT")
        krT2 = alloc([128, 4096], BF16, "krT2")
        qrT = alloc([128, 4, 2048], BF16, "qrT")
        mix_off = top[0]
        mixT = alloc([128, 8, 2048], BF16, "mixT")
        persist_top = top[0]

        def rope_tables(pos_t, cols, f0, nf, scale, temps, cs, sn):
            tt, ti, tf = temps
            n = len(cols)
            tta, tia, tfa = tt.ap[:, 0:n, 0:nf], ti.ap[:, 0:n, 0:nf], tf.ap[:, 0:n, 0:nf]
            for i, bl in enumerate(cols):
                TS("dve", tt.ap[:, i, 0:nf], invf.ap[:, f0:f0 + nf], pos_t.ap[:, bl:bl + 1], None, ALU.mult, None,
                   [invf.b, pos_t.b], [tt.b])
            CP("dve", tia, tta, [tt.b], [ti.b])
            CP("dve", tfa, tia, [ti.b], [tf.b])
            TT("dve", tfa, tta, tfa, ALU.subtract, [tt.b, tf.b], [tf.b])
            ACT(sn.ap, tfa, AF.Sin, [tf.b], [sn.b], scale=6.28318 * 1.0)
            TS("dve", tta, tta, 0.25, None, ALU.add, None, [tt.b], [tt.b])
            CP("dve", tia, tta, [tt.b], [ti.b])
            CP("dve", tfa, tia, [ti.b], [tf.b])
            TT("dve", tfa, tta, tfa, ALU.subtract, [tt.b, tf.b], [tf.b])
            ACT(cs.ap, tfa, AF.Sin, [tf.b], [cs.b], scale=6.28318 * 1.0)
            if scale != 1.0:
                TS("dve", cs.ap, cs.ap, scale, None, ALU.mult, None, [cs.b], [cs.b])
                TS("dve", sn.ap, sn.ap, scale, None, ALU.mult, None, [sn.b], [sn.b])

        def rotary(src, srcb, dst, dstb, cs, sn, csb, nh, half, tmp):
            x1 = src[:, :, 0:half]
            x2 = src[:, :, half:2 * half]
            cb = bc(cs, [128, nh, half], 1)
            sb_ = bc(sn, [128, nh, half], 1)
            t1, t2, t3, t4 = tmp
            a1 = t1.ap[:, 0:nh, 0:half]
            a2 = t2.ap[:, 0:nh, 0:half]
            a3 = t3.ap[:, 0:nh, 0:half]
            a4 = t4.ap[:, 0:nh, 0:half]
            TT("dve", a1, x1, cb, ALU.mult, srcb + csb, [t1.b])
            TT("dve", a2, x2, sb_, ALU.mult, srcb + csb, [t2.b])
            TT("dve", a3, x2, cb, ALU.mult, srcb + csb, [t3.b])
            TT("dve", a4, x1, sb_, ALU.mult, srcb + csb, [t4.b])
            TT("pool", dst[:, :, 0:half], a1, a2, ALU.subtract, [t1.b, t2.b], dstb)
            TT("pool", dst[:, :, half:2 * half], a3, a4, ALU.add, [t3.b, t4.b], dstb)

        def rms_scale(psrc, psb, nm, ntok, dst, dstb, inv_n, eps, sq, ssq_ps, ssq_pb, rr):
            ACT(sq.ap[:, 0:nm, 0:ntok], psrc, AF.Square, psb, [sq.b])
            for m in range(nm):
                MM(ssq_ps[:, 0:ntok], ones_b.ap, sq.ap[:, m, 0:ntok], m == 0, m == nm - 1, [ones_b.b, sq.b], [ssq_pb])
            ACT(rr.ap[:, 0:ntok], ssq_ps[:, 0:ntok], AF.Sqrt, [ssq_pb], [rr.b], bias=eps, scale=inv_n)
            S.add("dve", lambda e: e.reciprocal(out=rr.ap[:, 0:ntok], in_=rr.ap[:, 0:ntok]), [rr.b], [rr.b])
            TT("dve", dst, psrc, bc(rr.ap[:, 0:ntok], [128, nm, ntok], 1), ALU.mult, psb + [rr.b], dstb)

        wcq = alloc([128, 8, 384], BF16, "wcq")
        wckv = alloc([128, 8, 320], BF16, "wckv")
        wuq_f = alloc([128, 3, 1536], F32, "wuqf")
        wuq = alloc([128, 3, 1536], BF16, "wuq")
        qg = alloc([128, 3], F32, "qg")
        DMA("pool", wcq.ap, d_wcq, (), [wcq.b], "d_w0")
        DMA("pool", wckv.ap, d_wckv, (), [wckv.b], "d_w1")
        DMA("sp", wuq_f.ap, d_wuq, (), [wuq_f.b], "d_w2")
        DMA("sp", qg.ap, d_qg, (), [qg.b], "d_c4")
        for m in range(3):
            TS("dve", wuq.ap[:, m, :], wuq_f.ap[:, m, :], qg.ap[:, m:m + 1], None, ALU.mult, None,
               [wuq_f.b, qg.b], [wuq.b])
        rtmp = (alloc([128, 32, 32], F32, "rtt"), alloc([128, 32, 32], I32, "rti"), alloc([128, 32, 32], F32, "rtf"))
        cMo, sMo = alloc([128, 16, 32], F32, "cMo"), alloc([128, 16, 32], F32, "sMo")
        cMa, sMa = alloc([128, 32, 32], F32, "cMa"), alloc([128, 32, 32], F32, "sMa")
        rope_tables(posO, list(range(16)), 64, 32, SM_SCALE, rtmp, cMo, sMo)
        rope_tables(posA, list(range(32)), 64, 32, 1.0, rtmp, cMa, sMa)
        xa = [alloc([128, 8, 256], BF16, "xa") for _ in range(2)]
        xo = [alloc([128, 8, 128], BF16, "xo") for _ in range(2)]
        sq = alloc([128, 3, 256], BF16, "sq")
        rr = alloc([128, 256], F32, "rr")
        qr_tok = alloc([128, 8, 64], BF16, "qrtok")
        kr_tok = alloc([128, 2, 64], BF16, "krtok")
        tmp1 = tuple(alloc([128, 8, 32], F32, "t%d" % i) for i in range(4))
        for j in range(NPAIR):
            A = xa[j % 2]
            O = xo[j % 2]
            if j == 0:
                DMA("pool", A.ap, xT_all[:, :, 0:256], (), [A.b], "d_xa0")
                DMA("pool", O.ap, xT_own[:, :, 0:128], (), [O.b], "d_xo0")
            if j + 1 < NPAIR:
                An, On = xa[(j + 1) % 2], xo[(j + 1) % 2]
                DMA("pool", An.ap, xT_all[:, :, (j + 1) * 256:(j + 2) * 256], (), [An.b], "d_xa%d" % ((j + 1) % 2))
                DMA("pool", On.ap, xT_own[:, :, (j + 1) * 128:(j + 2) * 128], (), [On.b], "d_xo%d" % ((j + 1) % 2))
            pq = PS[0][:, 0:384].rearrange("p (m t) -> p m t", m=3)
            for m in range(3):
                for k in range(8):
                    MM(pq[:, m, :], wcq.ap[:, k, m * 128:(m + 1) * 128], O.ap[:, k, :], k == 0, k == 7,
                       [wcq.b, O.b], [PB[0]])
            rms_scale(pq, [PB[0]], 3, 128, cqnT.ap[:, :, j * 128:(j + 1) * 128], [cqnT.b], 1.0 / 384, 1e-6,
                      sq, PS[1], PB[1], rr)
            for m in range(3):
                MM(PS[2][:, :], cqnT.ap[:, m, j * 128:(j + 1) * 128], wuq.ap[:, m, 1024:1536], m == 0, m == 2,
                   [cqnT.b, wuq.b], [PB[2]])
            rotary(PS[2][:, :].rearrange("p (h d) -> p h d", h=8), [PB[2]], qr_tok.ap, [qr_tok.b],
                   cMo.ap[:, j, :], sMo.ap[:, j, :], [cMo.b, sMo.b], 8, 32, tmp1)
            pt = PS[3][:, 0:256].bitcast(BF16).rearrange("p (a t) -> p a t", a=4)
            for hp in range(4):
                TR(pt[:, hp, :], qr_tok.ap[:, 2 * hp:2 * hp + 2, :].rearrange("p a d -> p (a d)"), ident_b.ap,
                   [qr_tok.b, ident_b.b], [PB[3]])
            CP("act", qrT.ap[:, :, j * 128:(j + 1) * 128], pt, [PB[3]], [qrT.b])
            pk = PS[4][:, :].rearrange("p (m t) -> p m t", m=2)
            for m in range(2):
                for k in range(8):
                    MM(pk[:, m, :], wckv.ap[:, k, m * 128:(m + 1) * 128], A.ap[:, k, :], k == 0, k == 7,
                       [wckv.b, A.b], [PB[4]])
            rms_scale(pk, [PB[4]], 2, 256, ckvnT.ap[:, :, j * 256:(j + 1) * 256], [ckvnT.b], 1.0 / 256, 1e-6,
                      sq, PS[5], PB[5], rr)
            for X in range(2):
                blk = 2 * j + X
                for k in range(8):
                    MM(PS[6][:, 0:64], A.ap[:, k, X * 128:(X + 1) * 128], wckv.ap[:, k, 256:320], k == 0, k == 7,
                       [A.b, wckv.b], [PB[6]])
                rotary(PS[6][:, 0:64].rearrange("p (h d) -> p h d", h=1), [PB[6]], kr_tok.ap[:, 0:1, :], [kr_tok.b],
                       cMa.ap[:, blk, :], sMa.ap[:, blk, :], [cMa.b, sMa.b], 1, 32, tmp1)
                CP("pool", kr_tok.ap[:, 1, :], kr_tok.ap[:, 0, :], [kr_tok.b], [kr_tok.b])
                pt2 = PS[7][:, 0:64].bitcast(BF16)
                TR(pt2, kr_tok.ap.rearrange("p a d -> p (a d)"), ident_b.ap, [kr_tok.b, ident_b.b], [PB[7]])
                CP("act", krT2.ap[:, blk * 128:(blk + 1) * 128], pt2, [PB[7]], [krT2.b])

        S.barrier()
        top[0] = persist_top
        wrq = alloc([128, 8, 1024], BF16, "wrq")
        wrk = alloc([128, 8, 1024], BF16, "wrk")
        wrv = alloc([128, 8, 1024], BF16, "wrv")
        for i, (w, d) in enumerate(((wrq, d_wrq), (wrk, d_wrk), (wrv, d_wrv))):
            for hf in range(2):
                DMA("pool", w.ap[:, :, hf * 512:(hf + 1) * 512], d[:, :, hf * 512:(hf + 1) * 512], (), [w.b], "d_w%d" % (3 + i))
        tab = alloc([128, 2, 8, 128], F32, "tab")
        qwt = alloc([128, 8, 128], F32, "qwt")
        kwt = alloc([128, 2, 8], F32, "kwt")
        cd2 = alloc([128, 8], F32, "cd2")
        DMA("sp", tab.ap, d_tab, (), [tab.b], "d_c5")
        DMA("sp", qwt.ap, d_qw, (), [qwt.b], "d_c6")
        DMA("sp", kwt.ap, d_kw, (), [kwt.b], "d_c7")
        DMA("sp", cd2.ap, d_cd2, (), [cd2.b], "d_c8")
        rtmp = (alloc([128, 2, 64], F32, "rtt"), alloc([128, 2, 64], I32, "rti"), alloc([128, 2, 64], F32, "rtf"))
        cRo, sRo = alloc([128, 1, 64], F32, "cRo"), alloc([128, 1, 64], F32, "sRo")
        cRa, sRa = alloc([128, 2, 64], F32, "cRa"), alloc([128, 2, 64], F32, "sRa")
        xa = [alloc([128, 8, 256], BF16, "xa") for _ in range(2)]
        xo = [alloc([128, 8, 128], BF16, "xo") for _ in range(2)]
        tmp2 = tuple(alloc([128, 4, 64], F32, "t%d" % i) for i in range(4))
        k_tok = alloc([128, 8, 128], BF16, "ktok")
        kw_tok = alloc([128, 2, 8, 128], BF16, "kwtok")
        v_tok = alloc([128, 2, 8, 128], BF16, "vtok")
        q_tok = alloc([128, 8, 128], BF16, "qtok")
        kT = alloc([128, 2, 8, 128], BF16, "kT")
        qT = alloc([128, 8, 128], BF16, "qT")
        qwT = alloc([128, 8, 128], BF16, "qwT")
        sc = alloc([128, 8, 2, 128], BF16, "sc")
        Rf = alloc([128, 8, 128], F32, "Rf")
        Rb = alloc([128, 8, 128], BF16, "Rb")
        o_sb = alloc([128, 8, 128], F32, "osb")
        o_sq = alloc([128, 8, 128], BF16, "osq")
        gn_b = alloc([128, 8, 128], BF16, "gnb")
        st = alloc([128, 32], F32, "st")
        S.add("pool", lambda e: e.memset(Rf.ap, 0.0), (), [Rf.b])
        S.add("pool", lambda e: e.memset(Rb.ap, 0.0), (), [Rb.b])

        def proj_tok(xsrc, xb, w, pa, pb_):
            for hf in range(2):
                for k in range(8):
                    MM(PS[pa + hf][:, :], xsrc[:, k, :], w.ap[:, k, hf * 512:(hf + 1) * 512], k == 0, k == 7,
                       [xb, w.b], [PB[pa + hf]])

        def ps2(pa):
            return [PS[pa + hf][:, :].rearrange("p (h d) -> p h d", h=4) for hf in range(2)]

        for j in range(NPAIR):
            A = xa[j % 2]
            O = xo[j % 2]
            if j == 0:
                DMA("pool", A.ap, xT_all[:, :, 0:256], (), [A.b], "d_xa0")
                DMA("pool", O.ap, xT_own[:, :, 0:128], (), [O.b], "d_xo0")
            if j + 1 < NPAIR:
                An, On = xa[(j + 1) % 2], xo[(j + 1) % 2]
                DMA("pool", An.ap, xT_all[:, :, (j + 1) * 256:(j + 2) * 256], (), [An.b], "d_xa%d" % ((j + 1) % 2))
                DMA("pool", On.ap, xT_own[:, :, (j + 1) * 128:(j + 2) * 128], (), [On.b], "d_xo%d" % ((j + 1) % 2))
            rope_tables(posA, [2 * j, 2 * j + 1], 0, 64, 1.0, rtmp, cRa, sRa)
            rope_tables(posO, [j], 0, 64, 1.0, rtmp, cRo, sRo)
            for X in range(2):
                blk = 2 * j + X
                xs = A.ap[:, :, X * 128:(X + 1) * 128]
                proj_tok(xs, A.b, wrk, 0, None)
                pv = ps2(0)
                for hf in range(2):
                    rotary(pv[hf], [PB[hf]], k_tok.ap[:, hf * 4:(hf + 1) * 4, :], [k_tok.b],
                           cRa.ap[:, X, :], sRa.ap[:, X, :], [cRa.b, sRa.b], 4, 64, tmp2)
                TT("dve", kw_tok.ap[:, X, :, :], k_tok.ap, bc(kwt.ap[:, X, :], [128, 8, 128], 2), ALU.mult,
                   [k_tok.b, kwt.b], [kw_tok.b])
                ptk = PS[4][:, :].bitcast(BF16).rearrange("p (h t) -> p h t", h=8)
                for h in range(8):
                    TR(ptk[:, h, :], k_tok.ap[:, h, :], ident_b.ap, [k_tok.b, ident_b.b], [PB[4]])
                CP("act", kT.ap[:, X, :, :], ptk, [PB[4]], [kT.b])
                proj_tok(xs, A.b, wrv, 2, None)
                pv = ps2(2)
                for hf in range(2):
                    CP("act", v_tok.ap[:, X, hf * 4:(hf + 1) * 4, :], pv[hf], [PB[2 + hf]], [v_tok.b])
            proj_tok(O.ap, O.b, wrq, 0, None)
            pv = ps2(0)
            for hf in range(2):
                rotary(pv[hf], [PB[hf]], q_tok.ap[:, hf * 4:(hf + 1) * 4, :], [q_tok.b],
                       cRo.ap[:, 0, :], sRo.ap[:, 0, :], [cRo.b, sRo.b], 4, 64, tmp2)
            ptq = PS[4][:, :].bitcast(BF16).rearrange("p (h t) -> p h t", h=8)
            for h in range(8):
                TR(ptq[:, h, :], q_tok.ap[:, h, :], ident_b.ap, [q_tok.b, ident_b.b], [PB[4]])
            CP("act", qT.ap, ptq, [PB[4]], [qT.b])
            TT("dve", qwT.ap, qT.ap, qwt.ap, ALU.mult, [qT.b, qwt.b], [qwT.b])
            for h2 in range(4):
                pss = PS[5][:, :].rearrange("p (h x q) -> p h x q", h=2, x=2)
                for hh in range(2):
                    h = 2 * h2 + hh
                    for X in range(2):
                        MM(pss[:, hh, X, :], kT.ap[:, X, h, :], qT.ap[:, h, :], True, True, [kT.b, qT.b], [PB[5]])
                TT("dve", sc.ap[:, 2 * h2:2 * h2 + 2, :, :], pss,
                   tab.ap[:, :, 2 * h2:2 * h2 + 2, :].rearrange("p x h q -> p h x q"), ALU.mult,
                   [PB[5], tab.b], [sc.b])
            po = [PS[6][:, :].rearrange("p (h d) -> p h d", h=4), PS[7][:, :].rearrange("p (h d) -> p h d", h=4)]
            for h in range(8):
                o_ps = po[h // 4][:, h % 4, :]
                pbb = PB[6 + h // 4]
                MM(o_ps, sc.ap[:, h, 0, :], v_tok.ap[:, 0, h, :], True, False, [sc.b, v_tok.b], [pbb])
                MM(o_ps, sc.ap[:, h, 1, :], v_tok.ap[:, 1, h, :], False, False, [sc.b, v_tok.b], [pbb])
                MM(o_ps, qwT.ap[:, h, :], Rb.ap[:, h, :], False, True, [qwT.b, Rb.b], [pbb])
            for hf in range(2):
                CP("act", o_sb.ap[:, hf * 4:(hf + 1) * 4, :], po[hf], [PB[6 + hf]], [o_sb.b])
            S.add("dve", lambda e: e.tensor_reduce(out=st.ap[:, 0:8], in_=o_sb.ap, axis=AX.X, op=ALU.add), [o_sb.b], [st.b])
            TS("dve", st.ap[:, 0:8], st.ap[:, 0:8], 1.0 / 128, None, ALU.mult, None, [st.b], [st.b])
            TT("dve", o_sb.ap, o_sb.ap, bc(st.ap[:, 0:8], [128, 8, 128], 2), ALU.subtract, [o_sb.b, st.b], [o_sb.b])
            TT("pool", o_sq.ap, o_sb.ap, o_sb.ap, ALU.mult, [o_sb.b], [o_sq.b])
            S.add("dve", lambda e: e.tensor_reduce(out=st.ap[:, 8:16], in_=o_sq.ap, axis=AX.X, op=ALU.add), [o_sq.b], [st.b])
            ACT(st.ap[:, 16:24], st.ap[:, 8:16], AF.Sqrt, [st.b], [st.b], bias=1e-6, scale=1.0 / 128)
            S.add("dve", lambda e: e.reciprocal(out=st.ap[:, 16:24], in_=st.ap[:, 16:24]), [st.b], [st.b])
            TT("dve", gn_b.ap, o_sb.ap, bc(st.ap[:, 16:24], [128, 8, 128], 2), ALU.mult, [o_sb.b, st.b], [gn_b.b])
            ptg = PS[4][:, :].bitcast(BF16).rearrange("p (h t) -> p h t", h=8)
            for h in range(8):
                TR(ptg[:, h, :], gn_b.ap[:, h, :], ident_b.ap, [gn_b.b, ident_b.b], [PB[4]])
            CP("act", mixT.ap[:, :, j * 128:(j + 1) * 128], ptg, [PB[4]], [mixT.b])
            for h in range(8):
                u_ps = po[h // 4][:, h % 4, :]
                pbb = PB[6 + h // 4]
                MM(u_ps, kw_tok.ap[:, 0, h, :], v_tok.ap[:, 0, h, :], True, False, [kw_tok.b, v_tok.b], [pbb])
                MM(u_ps, kw_tok.ap[:, 1, h, :], v_tok.ap[:, 1, h, :], False, True, [kw_tok.b, v_tok.b], [pbb])
            TT("dve", Rf.ap, Rf.ap, bc(cd2.ap, [128, 8, 128], 2), ALU.mult, [Rf.b, cd2.b], [Rf.b])
            for hf in range(2):
                TT("dve", Rf.ap[:, hf * 4:(hf + 1) * 4, :], Rf.ap[:, hf * 4:(hf + 1) * 4, :], po[hf], ALU.add,
                   [Rf.b, PB[6 + hf]], [Rf.b])
            CP("act", Rb.ap, Rf.ap, [Rf.b], [Rb.b])

        S.barrier()
        top[0] = persist_top
        xTo = alloc([128, 8, 2048], BF16, "xTo")
        for q4 in range(4):
            DMA("pool", xTo.ap[:, :, q4 * 512:(q4 + 1) * 512], xT_own[:, :, q4 * 512:(q4 + 1) * 512], (), [xTo.b], "d_xto")
        wukv_f = alloc([128, 2, 2048], F32, "wukvf")
        wukv = alloc([128, 2, 2048], BF16, "wukv")
        kvg = alloc([128, 2], F32, "kvg")
        DMA("sp", wukv_f.ap, d_wukv, (), [wukv_f.b], "d_w6")
        DMA("sp", kvg.ap, d_kvg, (), [kvg.b], "d_c9")
        for m in range(2):
            TS("dve", wukv.ap[:, m, :], wukv_f.ap[:, m, :], kvg.ap[:, m:m + 1], None, ALU.mult, None,
               [wukv_f.b, kvg.b], [wukv.b])
        wuq_f = alloc([128, 3, 1024], F32, "wuqf")
        wuqn = alloc([128, 3, 1024], BF16, "wuqn")
        qg3 = alloc([128, 3], F32, "qg3")
        DMA("sp", wuq_f.ap, d_wuq[:, :, 0:1024], (), [wuq_f.b], "d_w2")
        DMA("sp", qg3.ap, d_qg, (), [qg3.b], "d_c4")
        for m in range(3):
            TS("dve", wuqn.ap[:, m, :], wuq_f.ap[:, m, :], qg3.ap[:, m:m + 1], None, ALU.mult, None,
               [wuq_f.b, qg3.b], [wuqn.b])
        wg = [alloc([128, 8, 384], BF16, "wg") for _ in range(2)]
        knT = alloc([128, 4096], BF16, "knT")
        Vh = alloc([128, 32, 128], BF16, "Vh")
        qnT = alloc([128, 2048], BF16, "qnT")
        pT = [alloc([128, 512], BF16, "pT") for _ in range(2)]
        rD = alloc([128, 512], F32, "rD")
        Pacc = alloc([128, 512], F32, "Pacc")
        ones_f = alloc([128, 128], F32, "onesf")
        S.add("pool", lambda e: e.memset(ones_f.ap, 1.0), (), [ones_f.b])
        o_f = rD
        g_mla = alloc([128, 512], F32, "gmla")
        g_ret = alloc([128, 512], F32, "gret")
        g_sil = alloc([128, 512], F32, "gsil")
        cnt = [0]
        for h in range(8):
            W = wg[h % 2]
            DMA("pool", W.ap, d_wgate[h], (), [W.b], "d_wg%d" % (h % 2))
            hp = (h % 2) * 64
            for tg in range(8):
                pb = tg % 2
                for m in range(2):
                    MM(PS[pb][:, :], wukv.ap[:, m, h * 256:h * 256 + 128], ckvnT.ap[:, m, tg * 512:(tg + 1) * 512],
                       m == 0, m == 1, [wukv.b, ckvnT.b], [PB[pb]])
                CP("act" if tg % 2 == 0 else "dve", knT.ap[:, tg * 512:(tg + 1) * 512], PS[pb][:, :], [PB[pb]], [knT.b])
            for b4 in range(8):
                pb = b4 % 2
                pvv = PS[pb][:, :].rearrange("p (a d) -> p a d", a=4)
                for a in range(4):
                    blk = b4 * 4 + a
                    for m in range(2):
                        MM(pvv[:, a, :], ckvnT.ap[:, m, blk * 128:(blk + 1) * 128],
                           wukv.ap[:, m, h * 256 + 128:h * 256 + 256], m == 0, m == 1, [ckvnT.b, wukv.b], [PB[pb]])
                CP("dve" if b4 % 2 == 0 else "act", Vh.ap[:, b4 * 4:(b4 + 1) * 4, :], pvv, [PB[pb]], [Vh.b])
            for g in range(4):
                pb = g % 2
                for m in range(3):
                    MM(PS[pb][:, :], wuqn.ap[:, m, h * 128:(h + 1) * 128], cqnT.ap[:, m, g * 512:(g + 1) * 512],
                       m == 0, m == 2, [wuqn.b, cqnT.b], [PB[pb]])
                ACT(qnT.ap[:, g * 512:(g + 1) * 512], PS[pb][:, :], AF.Copy, [PB[pb]], [qnT.b], scale=SM_SCALE)
            for g in range(4):
                gs = slice(g * 512, (g + 1) * 512)
                for t3, (gt, fn) in enumerate(((g_mla, AF.Sigmoid), (g_ret, AF.Sigmoid), (g_sil, AF.Silu))):
                    pb = 2 + (t3 % 2)
                    for k in range(8):
                        MM(PS[pb][:, :], W.ap[:, k, t3 * 128:(t3 + 1) * 128], xTo.ap[:, k, gs], k == 0, k == 7,
                           [W.b, xTo.b], [PB[pb]])
                    ACT(gt.ap, PS[pb][:, :], fn, [PB[pb]], [gt.b])
                TT("pool", g_ret.ap, g_ret.ap, g_sil.ap, ALU.mult, [g_ret.b, g_sil.b], [g_ret.b])
                TT("pool", g_ret.ap, g_ret.ap, mixT.ap[:, h, gs], ALU.mult, [g_ret.b, mixT.b], [g_ret.b])
                nkb = 8 * g + 8

                def kb_info(kb):
                    if kb < 8 * g:
                        return 0, None
                    ip = (kb - 8 * g) // 2
                    return ip * 128, (kb - 8 * g) % 2

                def s_stage(kb, slot):
                    c0, diag = kb_info(kb)
                    cs_ = slice(c0, 512)
                    qs_ = slice(g * 512 + c0, (g + 1) * 512)
                    sb_i = 4 + slot
                    MM(PS[sb_i][:, cs_], knT.ap[:, kb * 128:(kb + 1) * 128], qnT.ap[:, qs_], True, False,
                       [knT.b, qnT.b], [PB[sb_i]])
                    MM(PS[sb_i][:, cs_], krT2.ap[hp:hp + 64, kb * 128:(kb + 1) * 128], qrT.ap[hp:hp + 64, h // 2, qs_],
                       False, True, [krT2.b, qrT.b], [PB[sb_i]])

                def e_stage(kb, slot):
                    c0, diag = kb_info(kb)
                    cs_ = slice(c0, 512)
                    sb_i = 4 + slot
                    P = pT[slot]
                    ACT(P.ap[:, cs_], PS[sb_i][:, cs_], AF.Exp, [PB[sb_i]], [P.b])
                    if diag is not None:
                        TT("dve", P.ap[:, c0:c0 + 128], P.ap[:, c0:c0 + 128], mask.ap[:, diag, :], ALU.mult,
                           [P.b, mask.b], [P.b])
                    if kb == 0:
                        CP("dve", Pacc.ap[:, cs_], P.ap[:, cs_], [P.b], [Pacc.b])
                    else:
                        TT("dve", Pacc.ap[:, cs_], Pacc.ap[:, cs_], P.ap[:, cs_], ALU.add, [Pacc.b, P.b], [Pacc.b])

                def pv_stage(kb, slot):
                    c0, diag = kb_info(kb)
                    cs_ = slice(c0, 512)
                    P = pT[slot]
                    MM(PS[6][:, cs_], Vh.ap[:, kb, :], P.ap[:, cs_], kb == 0, kb == nkb - 1, [Vh.b, P.b], [PB[6]])

                base = cnt[0]
                cnt[0] += nkb
                s_stage(0, base % 2)
                for kb in range(nkb):
                    if kb + 1 < nkb:
                        s_stage(kb + 1, (base + kb + 1) % 2)
                    e_stage(kb, (base + kb) % 2)
                    pv_stage(kb, (base + kb) % 2)
                MM(PS[7][:, :], ones_f.ap, Pacc.ap, True, True, [ones_f.b, Pacc.b], [PB[7]])
                S.add("dve", lambda e: e.reciprocal(out=rD.ap, in_=PS[7][:, :]), [PB[7]], [rD.b])
                TT("dve", o_f.ap, PS[6][:, :], rD.ap, ALU.mult, [PB[6], rD.b], [o_f.b])
                TT("pool", o_f.ap, o_f.ap, g_mla.ap, ALU.mult, [o_f.b, g_mla.b], [o_f.b])
                TT("pool", mixT.ap[:, h, gs], o_f.ap, g_ret.ap, ALU.add, [o_f.b, g_ret.b], [mixT.b])

        S.barrier()
        top[0] = persist_top
        p4_top = top[0]
        top[0] = latent_off
        h1b = alloc([128, 16, 1024], BF16, "h1b")
        G = alloc([128, 16, 32], F32, "G")
        G17 = alloc([128, 32], F32, "G17")
        Ghl = alloc([128, 16, 32, 2], BF16, "Ghl")
        RK = alloc([128, 16, 32], F32, "RK")
        MKb = alloc([128, 16, 32], BF16, "MKb")
        Lst = alloc([128, 128], BF16, "Lst")
        iotaC = alloc([128, 128], F32, "iotaC")
        assert top[0] <= mix_off
        top[0] = p4_top
        acc = alloc([128, 16, 1024], F32, "acc")
        sm = alloc([128, 8], F32, "sm")
        junk = alloc([128, 1024], BF16, "junk")
        p5_keep = top[0]
        lnp = alloc([128, 2, 1024], F32, "lnp")
        for i in range(2):
            DMA("sp", lnp.ap[:, i, :], d_ln[i:i + 1, :].partition_broadcast(128), (), [lnp.b], "d_ln")
        Lf = alloc([128, 384], F32, "Lf")
        S.add("pool", lambda e: e.iota(Lf.ap[:, 0:128], [[1, 128]], base=0, channel_multiplier=-1,
                                       allow_small_or_imprecise_dtypes=True), (), [Lf.b])
        S.add("dve", lambda e: e.tensor_single_scalar(out=Lst.ap, in_=Lf.ap[:, 0:128], scalar=0.0, op=ALU.is_gt),
              [Lf.b], [Lst.b])
        S.add("pool", lambda e: e.iota(iotaC.ap, [[1, 128]], base=0, channel_multiplier=0,
                                       allow_small_or_imprecise_dtypes=True), (), [iotaC.b])
        wo = alloc([128, 8, 1024], BF16, "wo")
        for hf in range(2):
            DMA("pool", wo.ap[:, :, hf * 512:(hf + 1) * 512], d_wo[:, :, hf * 512:(hf + 1) * 512], (), [wo.b], "d_wo")
        wr = alloc([128, 8, 32], F32, "wr")
        brr = alloc([128, 32], F32, "brr")
        bdn = alloc([32, 1024], F32, "bdn")
        DMA("sp", wr.ap, d_wr, (), [wr.b], "d_c10")
        DMA("sp", brr.ap, d_br.partition_broadcast(128), (), [brr.b], "d_c11")
        DMA("sp", bdn.ap, d_bdn, (), [bdn.b], "d_c12")
        xres = [alloc([128, 1024], F32, "xres") for _ in range(2)]
        vv = alloc([128, 1024], F32, "vv")
        h1 = alloc([128, 1024], F32, "h1")
        h1Tf = alloc([128, 8, 128], F32, "h1Tf")
        lg = alloc([128, 32], F32, "lg")
        em = alloc([128, 32], F32, "em")
        mk = alloc([128, 32], F32, "mk")
        t8 = alloc([128, 8], F32, "t8")
        GT = alloc([32, 128], F32, "GT")

        def layernorm(src, srcb, dst, dstb, gi, bi):
            S.add("dve", lambda e: e.tensor_reduce(out=sm.ap[:, 0:1], in_=src, axis=AX.X, op=ALU.add), srcb, [sm.b])
            TS("dve", sm.ap[:, 0:1], sm.ap[:, 0:1], -1.0 / 1024, None, ALU.mult, None, [sm.b], [sm.b])
            ACT(src, src, AF.Identity, srcb + [sm.b], srcb, bias=sm.ap[:, 0:1])
            ACT(junk.ap, src, AF.Square, srcb, [junk.b, sm.b], accum=sm.ap[:, 1:2])
            ACT(sm.ap[:, 2:3], sm.ap[:, 1:2], AF.Sqrt, [sm.b], [sm.b], bias=1e-5, scale=1.0 / 1024)
            S.add("dve", lambda e: e.reciprocal(out=sm.ap[:, 2:3], in_=sm.ap[:, 2:3]), [sm.b], [sm.b])
            STT("dve", dst, src, sm.ap[:, 2:3], lnp.ap[:, gi, :], ALU.mult, ALU.mult, srcb + [sm.b, lnp.b], dstb)
            TT("pool", dst, dst, lnp.ap[:, bi, :], ALU.add, dstb + [lnp.b], dstb)

        for j in range(16):
            XR = xres[j % 2]
            DMA("sp", XR.ap, x_own[j], (), [XR.b], "d_xr%d" % (j % 2))
            for hf in range(2):
                for c in range(8):
                    MM(PS[hf][:, :], mixT.ap[:, c, j * 128:(j + 1) * 128], wo.ap[:, c, hf * 512:(hf + 1) * 512],
                       c == 0, c == 7, [mixT.b, wo.b], [PB[hf]])
            for hf in range(2):
                STT("dve", vv.ap[:, hf * 512:(hf + 1) * 512], XR.ap[:, hf * 512:(hf + 1) * 512], DN_ALPHA, PS[hf][:, :],
                    ALU.mult, ALU.add, [XR.b, PB[hf]], [vv.b])
            S.add("act", lambda e: e.memzero(sm.ap), [sm.b], [sm.b])
            layernorm(vv.ap, [vv.b], h1.ap, [h1.b], 0, 1)
            for c4 in range(2):
                ptf = PS[2 + c4][:, :].rearrange("p (a t) -> p a t", a=4)
                for a in range(4):
                    c = c4 * 4 + a
                    TR(ptf[:, a, :], h1.ap[:, c * 128:(c + 1) * 128], ident_f.ap, [h1.b, ident_f.b], [PB[2 + c4]])
                CP("act", h1Tf.ap[:, c4 * 4:(c4 + 1) * 4, :], ptf, [PB[2 + c4]], [h1Tf.b])
            CP("pool", h1b.ap[:, j, :], h1.ap, [h1.b], [h1b.b])
            for c in range(8):
                MM(PS[4][:, 0:32], h1Tf.ap[:, c, :], wr.ap[:, c, :], c == 0, c == 7, [h1Tf.b, wr.b], [PB[4]])
            TT("dve", lg.ap, PS[4][:, 0:32], brr.ap, ALU.add, [PB[4], brr.b], [lg.b])
            S.add("dve", lambda e: e.max(out=t8.ap, in_=lg.ap), [lg.b], [t8.b])
            TS("dve", mk.ap, lg.ap, t8.ap[:, 3:4], None, ALU.is_ge, None, [lg.b, t8.b], [mk.b])
            TS("dve", sm.ap[:, 4:5], t8.ap[:, 0:1], -1.0, None, ALU.mult, None, [t8.b], [sm.b])
            ACT(em.ap, lg.ap, AF.Exp, [lg.b, sm.b], [em.b], bias=sm.ap[:, 4:5])
            TT("dve", em.ap, em.ap, mk.ap, ALU.mult, [em.b, mk.b], [em.b])
            S.add("dve", lambda e: e.tensor_reduce(out=sm.ap[:, 5:6], in_=em.ap, axis=AX.X, op=ALU.add), [em.b], [sm.b])
            S.add("dve", lambda e: e.reciprocal(out=sm.ap[:, 5:6], in_=sm.ap[:, 5:6]), [sm.b], [sm.b])
            TS("dve", G.ap[:, j, :], em.ap, sm.ap[:, 5:6], None, ALU.mult, None, [em.b, sm.b], [G.b])
            TS("dve", G17.ap, em.ap, sm.ap[:, 5:6], 1.0 / 1.702, ALU.mult, ALU.mult, [em.b, sm.b], [G17.b])
            CP("dve", MKb.ap[:, j, :], mk.ap, [mk.b], [MKb.b])
            CP("dve", Ghl.ap[:, j, :, 0], G17.ap, [G17.b], [Ghl.b])
            TT("dve", G17.ap, G17.ap, Ghl.ap[:, j, :, 0], ALU.subtract, [G17.b, Ghl.b], [G17.b])
            CP("dve", Ghl.ap[:, j, :, 1], G17.ap, [G17.b], [Ghl.b])
            TR(PS[5][0:32, 0:128], G.ap[:, j, :], ident_f.ap, [G.b, ident_f.b], [PB[5]])
            CP("act", GT.ap, PS[5][0:32, 0:128], [PB[5]], [GT.b])
            for hf in range(2):
                MM(PS[6 + hf][:, :], GT.ap, bdn.ap[:, hf * 512:(hf + 1) * 512], True, True, [GT.b, bdn.b], [PB[6 + hf]])
                STT("dve", acc.ap[:, j, hf * 512:(hf + 1) * 512], h1.ap[:, hf * 512:(hf + 1) * 512], DN_ALPHA,
                    PS[6 + hf][:, :], ALU.mult, ALU.add, [h1.b, PB[6 + hf]], [acc.b])

        prk = PS[0][:, :].rearrange("p (j e) -> p j e", j=16)
        for j in range(16):
            nprev = j % 4
            MM(prk[:, j, :], Lst.ap, MKb.ap[:, j, :], True, nprev == 0, [Lst.b, MKb.b], [PB[0]])
            for i2 in range(nprev):
                j2 = (j // 4) * 4 + i2
                MM(prk[:, j, :], ones_b.ap, MKb.ap[:, j2, :], False, i2 == nprev - 1, [ones_b.b, MKb.b], [PB[0]])
        CP("dve", RK.ap, prk, [PB[0]], [RK.b])
        S.barrier()
        C = 512
        top[0] = mix_off
        Pm = alloc([128, 16, 128], BF16, "Pm")
        XeT = alloc([128, 8, C], BF16, "XeT")
        PT = alloc([128, 4, 512], BF16, "PT")
        aT = alloc([128, 8, C], BF16, "aT")
        Ye = alloc([128, 4, 1024], BF16, "Ye")
        assert top[0] <= persist_top
        top[0] = p5_keep
        bup = alloc([128, 32, 16], F32, "bup")
        DMA("sp", bup.ap, d_bup, (), [bup.b], "d_c13")
        bl1 = bup.ap.rearrange("p e (q s) -> p e q s", s=4)[:, :, :, 2:4]
        TS("dve", bl1, bl1, 1.0, None, ALU.add, None, [bup.b], [bup.b])
        wu = [alloc([128, 8, 512], BF16, "wu") for _ in range(2)]
        wd = [alloc([128, 2, 1024], BF16, "wd") for _ in range(4)]
        HG = alloc([128, C], F32, "hg")
        SG = alloc([128, C], F32, "sg")
        HL = alloc([128, C], F32, "hl")
        gsl = alloc([128, 4], F32, "gsl")
        wcnt = [0]
        dcnt = [0]
        scnt = [0]

        def issue_wu(ex, q):
            Wq = wu[wcnt[0] % 2]
            DMA("pool", Wq.ap, d_wup[ex, q], (), [Wq.b], "d_wu%d" % (wcnt[0] % 2))
            wcnt[0] += 1
            return Wq

        PT2 = [PT, alloc([128, 4, 512], BF16, "PTb")]
        gsl2 = [gsl, alloc([128, 4], F32, "gslb")]

        def onehot(ex):
            for j in range(16):
                TS("dve", Pm.ap[:, j, :], iotaC.ap, RK.ap[:, j, ex:ex + 1], G.ap[:, j, ex:ex + 1],
                   ALU.is_equal, ALU.mult, [iotaC.b, RK.b, G.b], [Pm.b])
            TS("dve", Pm.ap, Pm.ap, 0.0, None, ALU.is_gt, None, [Pm.b], [Pm.b])

        def gather(ex):
            for dc in range(8):
                pb = dc % 2
                for sbk in range(4):
                    for i2 in range(4):
                        j = sbk * 4 + i2
                        MM(PS[pb][:, sbk * 128:(sbk + 1) * 128], h1b.ap[:, j, dc * 128:(dc + 1) * 128], Pm.ap[:, j, :],
                           i2 == 0, i2 == 3, [h1b.b, Pm.b], [PB[pb]])
                CP("act", XeT.ap[:, dc, :], PS[pb][:, :], [PB[pb]], [XeT.b])

        def trans(ex):
            PTe = PT2[ex % 2]
            gse = gsl2[ex % 2]
            for s2 in range(2):
                pb = 2 + s2
                ptp = PS[pb][:, :].bitcast(BF16).rearrange("p (a t) -> p a t", a=8)
                for a in range(8):
                    j = s2 * 8 + a
                    TR(ptp[:, a, :], Pm.ap[:, j, :], ident_b.ap, [Pm.b, ident_b.b], [PB[pb]])
                CP("act", PTe.ap[:, s2 * 2:s2 * 2 + 2, :].rearrange("p a t -> p (a t)"), ptp.rearrange("p a t -> p (a t)"),
                   [PB[pb]], [PTe.b])
            pgs = PS[7][:, 0:8].rearrange("p (r two) -> p r two", r=4)
            for sbk in range(4):
                for i2 in range(4):
                    j = sbk * 4 + i2
                    MM(pgs[:, sbk, :], Pm.ap[:, j, :], Ghl.ap[:, j, ex, :], i2 == 0, i2 == 3, [Pm.b, Ghl.b], [PB[7]])
            S.add("dve", lambda e: e.tensor_reduce(out=gse.ap, in_=pgs, axis=AX.X, op=ALU.add), [PB[7]], [gse.b])

        def up(ex):
            wds = []
            for qd in range(4):
                Wd = wd[dcnt[0] % 4]
                DMA("pool", Wd.ap, d_wdn[ex, qd // 2][:, (qd % 2) * 2:(qd % 2) * 2 + 2, :], (), [Wd.b], "d_wd%d" % (dcnt[0] % 4))
                dcnt[0] += 1
                wds.append(Wd)
            for q in range(4):
                Wq = issue_wu(ex, q)
                for c in range(2):
                    i2 = scnt[0] % 2
                    scnt[0] += 1
                    pg = 2 * i2
                    pl = 2 * i2 + 1
                    for k in range(8):
                        MM(PS[pg][:, :], Wq.ap[:, k, c * 128:(c + 1) * 128], XeT.ap[:, k, :], k == 0, k == 7,
                           [Wq.b, XeT.b], [PB[pg]])
                    for k in range(8):
                        MM(PS[pl][:, :], Wq.ap[:, k, (2 + c) * 128:(3 + c) * 128], XeT.ap[:, k, :], k == 0, k == 7,
                           [Wq.b, XeT.b], [PB[pl]])
                    TS("dve", HG.ap, PS[pg][:, :], bup.ap[:, ex, q * 4 + c:q * 4 + c + 1], 7.0, ALU.add, ALU.min,
                       [PB[pg], bup.b], [HG.b])
                    ACT(SG.ap, HG.ap, AF.Silu, [HG.b], [SG.b], scale=1.702)
                    TS("dve", HL.ap, PS[pl][:, :], bup.ap[:, ex, q * 4 + 2 + c:q * 4 + 3 + c], 8.0, ALU.add, ALU.min,
                       [PB[pl], bup.b], [HL.b])
                    STT("dve", aT.ap[:, 2 * q + c, :], HL.ap, -6.0, SG.ap, ALU.max, ALU.mult, [HL.b, SG.b], [aT.b])
            return wds

        def down(ex, wds):
            gse = gsl2[ex % 2]
            for rc in range(4):
                for hf in range(2):
                    pb = 4 + (rc % 2) * 2 + hf
                    for fc in range(8):
                        MM(PS[pb][:, :], aT.ap[:, fc, rc * 128:(rc + 1) * 128],
                           wds[fc // 2].ap[:, fc % 2, hf * 512:(hf + 1) * 512], fc == 0, fc == 7,
                           [aT.b, wds[fc // 2].b], [PB[pb]])
                    ACT(Ye.ap[:, rc, hf * 512:(hf + 1) * 512], PS[pb][:, :], AF.Copy, [PB[pb], gse.b], [Ye.b],
                        scale=gse.ap[:, rc:rc + 1])

        def scatter(ex):
            PTe = PT2[ex % 2]
            for j in range(16):
                sbk = j // 4
                for hf in range(2):
                    pb = 4 + ((j * 2 + hf) % 4)
                    MM(PS[pb][:, :], PTe.ap[:, sbk, (j % 4) * 128:(j % 4 + 1) * 128], Ye.ap[:, sbk, hf * 512:(hf + 1) * 512],
                       True, True, [PTe.b, Ye.b], [PB[pb]])
                    TT("dve", acc.ap[:, j, hf * 512:(hf + 1) * 512], acc.ap[:, j, hf * 512:(hf + 1) * 512], PS[pb][:, :],
                       ALU.add, [acc.b, PB[pb]], [acc.b])

        onehot(0)
        gather(0)
        trans(0)
        for ex in range(32):
            if ex + 1 < 32:
                onehot(ex + 1)
            wds = up(ex)
            if ex + 1 < 32:
                gather(ex + 1)
                trans(ex + 1)
            down(ex, wds)
            scatter(ex)
        S.barrier()
        top[0] = p5_keep
        lnp = alloc([128, 2, 1024], F32, "lnp")
        for i in range(2):
            DMA("sp", lnp.ap[:, i, :], d_ln[2 + i:3 + i, :].partition_broadcast(128), (), [lnp.b], "d_ln")
        for blk in range(16):
            S.add("act", lambda e: e.memzero(sm.ap), [sm.b], [sm.b])
            layernorm(acc.ap[:, blk, :], [acc.b], acc.ap[:, blk, :], [acc.b], 0, 1)
            DMA("sp", out_d[blk], acc.ap[:, blk, :], [acc.b], (), "d_out")

        S.emit(es, final_dma_sems=["d_out"])
    return nc


def _tile_k(w):
    k, n = w.shape
    return np.ascontiguousarray(w.reshape(k // 128, 128, n).transpose(1, 0, 2))


_PROG = {}


def _host_tables(p):
    H = 8
    gam = 1.0 - 2.0 ** (-5.0 - np.arange(H, dtype=np.float64))
    lg = np.log(gam)
    i = np.arange(128, dtype=np.float64)
    i_abs = i + 128 * p
    dk = 128.0 ** -0.5
    tab = np.zeros((128, 2, H, 128), np.float64)
    for X in range(2):
        j_abs = i + 128 * X
        rel = i_abs[None, :] - j_abs[:, None]
        for h in range(H):
            tab[:, X, h, :] = np.where(rel >= 0, np.exp(np.maximum(rel, 0) * lg[h]), 0.0) * dk
    qw = np.zeros((128, H, 128), np.float64)
    for h in range(H):
        qw[:, h, :] = np.exp((i_abs + 1.0) * lg[h])[None, :]
    kw = np.zeros((128, 2, H), np.float64)
    for X in range(2):
        for h in range(H):
            kw[:, X, h] = np.exp((255.0 - (i + 128 * X)) * lg[h]) * dk
    cd2 = np.broadcast_to(np.exp(256.0 * lg)[None, :], (128, H))
    causal = (i[None, :] >= i[:, None]).astype(np.float64)
    mask = np.zeros((128, 2, 128), np.float64)
    if p == 0:
        mask[:, 0, :] = causal
        mask[:, 1, :] = 0.0
    else:
        mask[:, 0, :] = 1.0
        mask[:, 1, :] = causal
    f32 = lambda a: np.ascontiguousarray(a, dtype=np.float32)
    return {"tab": f32(tab), "qwt": f32(qw), "kwt": f32(kw), "cd2": f32(cd2), "mask": f32(mask)}


def kernel(x, positions, w_in, q_norm_g, w_uq, kv_norm_g, w_ukv, w_o, ln1_g, ln1_b,
           w_router, b_router, w_up, b_up, w_down, b_down, ln2_g, ln2_b):
    f = lambda a: np.asarray(a, dtype=np.float32)
    x = f(x)
    positions = np.asarray(positions, dtype=np.int32)
    w_in = f(w_in)[0]
    shared = {}
    shared["wcq"] = _tile_k(w_in[:, 0:384])
    shared["wckv"] = _tile_k(w_in[:, 384:704])
    shared["wrq"] = _tile_k(w_in[:, 704:1728])
    shared["wrk"] = _tile_k(w_in[:, 1728:2752])
    shared["wrv"] = _tile_k(w_in[:, 2752:3776])
    r_g = w_in[:, 3776:4800]
    g_mla = w_in[:, 4800:5824]
    g_ret = w_in[:, 5824:6848]
    shared["wgate"] = np.stack([
        _tile_k(np.concatenate([g_mla[:, h * 128:(h + 1) * 128], g_ret[:, h * 128:(h + 1) * 128],
                                r_g[:, h * 128:(h + 1) * 128]], axis=1)) for h in range(8)])
    wuq = f(w_uq)[0].reshape(384, 8, 192)
    shared["wuq"] = _tile_k(np.concatenate([wuq[:, :, 0:128].reshape(384, 1024), wuq[:, :, 128:192].reshape(384, 512)], axis=1))
    shared["qg"] = np.ascontiguousarray(f(q_norm_g)[0].reshape(3, 128).T)
    shared["wukv"] = _tile_k(f(w_ukv)[0])
    shared["kvg"] = np.ascontiguousarray(f(kv_norm_g)[0].reshape(2, 128).T)
    shared["wo"] = _tile_k(f(w_o)[0])
    shared["lnp"] = np.ascontiguousarray(np.stack([f(ln1_g)[0], f(ln1_b)[0], f(ln2_g)[0], f(ln2_b)[0]]))
    shared["wr"] = _tile_k(f(w_router)[0])
    shared["br"] = np.ascontiguousarray(f(b_router)[0][None, :])
    wu = f(w_up)[0]
    wu = wu.reshape(32, 8, 128, 2, 4, 256)
    wu = wu.transpose(0, 4, 2, 1, 3, 5)
    shared["wup"] = np.ascontiguousarray(wu.reshape(32, 4, 128, 8, 512))
    bu = f(b_up)[0].reshape(32, 2, 4, 2, 128)
    shared["bup"] = np.ascontiguousarray(bu.transpose(4, 0, 2, 1, 3).reshape(128, 32, 16))
    wd = f(w_down)[0].reshape(32, 2, 4, 128, 1024)
    shared["wdn"] = np.ascontiguousarray(wd.transpose(0, 1, 3, 2, 4))
    shared["bdn"] = np.ascontiguousarray(f(b_down)[0])
    inv_r = (1.0 / (np.float32(10000.0) ** np.linspace(0.0, 1.0, 64, dtype=np.float32))).astype(np.float64)
    inv_m = (1.0 / (np.float32(10000.0) ** (np.arange(0, 64, 2, dtype=np.float32) / np.float32(64)))).astype(np.float64)
    invf = np.concatenate([inv_r, inv_m]) / (2.0 * np.pi)
    shared["invf"] = np.ascontiguousarray(np.broadcast_to(invf[None, :], (128, 96)), dtype=np.float32)
    tabs = [_host_tables(0), _host_tables(1)]
    in_maps = []
    for c in range(8):
        b, p = c // 2, c % 2
        xb = x[b]
        xo = xb.reshape(32, 128, 1024)[p::2]
        m = dict(shared)
        m.update(tabs[p])
        m["xT_all"] = _tile_k(np.ascontiguousarray(xb.T))
        m["xT_own"] = _tile_k(np.ascontiguousarray(xo.reshape(2048, 1024).T))
        m["x_own"] = np.ascontiguousarray(xo)
        pb = positions[b].reshape(32, 128)
        m["pos_all"] = np.ascontiguousarray(pb.T)
        m["pos_own"] = np.ascontiguousarray(pb[p::2].T)
        in_maps.append(m)
    if "nc" not in _PROG:
        _PROG["nc"] = build_program()
    res = run_bass_kernel_spmd(_PROG["nc"], in_maps, core_ids=list(range(8)))
    out = np.zeros((4, 32, 128, 1024), np.float32)
    for c in range(8):
        b, p = c // 2, c % 2
        out[b, p::2] = res.results[c]["out"]
    return out.reshape(4, 4096, 1024)
```

```python
import numpy as np
import concourse.bass as bass
import concourse.mybir as mybir
from concourse.bass_utils import run_bass_kernel_spmd
from contextlib import ExitStack

F32 = mybir.dt.float32
BF16 = mybir.dt.bfloat16
I32 = mybir.dt.int32
ALU = mybir.AluOpType
AF = mybir.ActivationFunctionType
AX = mybir.AxisListType

ENGS = ["pe", "act", "dve", "pool", "sp"]
NPAIR = 16
DN_ALPHA = 2.0 ** 0.25
SM_SCALE = 192.0 ** -0.5


class Buf:
    def __init__(self, name):
        self.name = name
        self.w = {}
        self.r = {}


class Sched:
    def __init__(self, nc):
        self.nc = nc
        self.ops = {e: [] for e in ENGS}
        self.events = {}
        self.known = {e: {} for e in ENGS}
        self.pending = {e: [] for e in ENGS}

    def add(self, eng, fn, reads=(), writes=(), dsem=None):
        semkey = dsem if dsem is not None else eng
        rec = {"eng": eng, "fn": fn, "waits": [], "inc": dsem is not None, "semkey": semkey,
               "is_dma": dsem is not None}
        evs = self.events.setdefault(semkey, [])
        evs.append(rec)
        rec["ord"] = len(evs)
        need = {}
        for b in reads:
            for k, (o, r) in b.w.items():
                if k not in need or need[k][0] < o:
                    need[k] = (o, r)
        for b in writes:
            for d in (b.w, b.r):
                for k, (o, r) in d.items():
                    if k not in need or need[k][0] < o:
                        need[k] = (o, r)
        for (k, o, r) in self.pending[eng]:
            if k not in need or need[k][0] < o:
                need[k] = (o, r)
        self.pending[eng] = []
        kn = self.known[eng]
        for k, (o, r) in need.items():
            if r is rec:
                continue
            if k == eng and not rec["is_dma"] and eng == "pe":
                continue
            if kn.get(k, 0) >= o:
                continue
            kn[k] = o
            r["inc"] = True
            rec["waits"].append(r)
        for b in reads:
            b.r[semkey] = (rec["ord"], rec)
        for b in writes:
            b.w[semkey] = (rec["ord"], rec)
        self.ops[eng].append(rec)
        return rec

    def barrier(self):
        last = []
        for k, evs in self.events.items():
            if evs:
                last.append((k, evs[-1]["ord"], evs[-1]))
        for e in ENGS:
            self.pending[e] = list(last)

    def emit(self, es, final_dma_sems=()):
        nc = self.nc
        sems = {}
        for k in self.events:
            sems[k] = es.enter_context(nc.semaphore("s_" + k))
        for k, evs in self.events.items():
            c = 0
            for r in evs:
                if r["is_dma"]:
                    c += 16
                    r["val"] = c
                elif r["inc"]:
                    c += 1
                    r["val"] = c
        ops = self.ops
        events = self.events

        def run(engname, eh):
            for r in ops[engname]:
                for w in r["waits"]:
                    eh.wait_ge(sems[w["semkey"]], w["val"])
                ins = r["fn"](eh)
                if r["is_dma"]:
                    ins.then_inc(sems[r["semkey"]], 16)
                elif r["inc"]:
                    ins.then_inc(sems[r["semkey"]], 1)
            if engname == "sp":
                for k in final_dma_sems:
                    evs = events.get(k)
                    if evs:
                        eh.wait_ge(sems[k], evs[-1]["val"])

        with nc.Block() as block:
            @block.tensor
            def _(e):
                run("pe", e)

            @block.scalar
            def _(e):
                run("act", e)

            @block.vector
            def _(e):
                run("dve", e)

            @block.gpsimd
            def _(e):
                run("pool", e)

            @block.sync
            def _(e):
                run("sp", e)


def build_program(debug=False):
    nc = bass.Bass("TRN2", target_bir_lowering=False)

    def din(name, shape, dt=F32):
        return nc.dram_tensor(name, list(shape), dt, kind="ExternalInput").ap()

    xT_all = din("xT_all", [128, 8, 4096])
    xT_own = din("xT_own", [128, 8, 2048])
    x_own = din("x_own", [16, 128, 1024])
    pos_all = din("pos_all", [128, 32], I32)
    pos_own = din("pos_own", [128, 16], I32)
    d_wcq = din("wcq", [128, 8, 384])
    d_wckv = din("wckv", [128, 8, 320])
    d_wrq = din("wrq", [128, 8, 1024])
    d_wrk = din("wrk", [128, 8, 1024])
    d_wrv = din("wrv", [128, 8, 1024])
    d_wgate = din("wgate", [8, 128, 8, 384])
    d_wuq = din("wuq", [128, 3, 1536])
    d_qg = din("qg", [128, 3])
    d_wukv = din("wukv", [128, 2, 2048])
    d_kvg = din("kvg", [128, 2])
    d_wo = din("wo", [128, 8, 1024])
    d_ln = din("lnp", [4, 1024])
    d_wr = din("wr", [128, 8, 32])
    d_br = din("br", [1, 32])
    d_wup = din("wup", [32, 4, 128, 8, 512])
    d_bup = din("bup", [128, 32, 16])
    d_wdn = din("wdn", [32, 2, 128, 4, 1024])
    d_bdn = din("bdn", [32, 1024])
    d_tab = din("tab", [128, 2, 8, 128])
    d_qw = din("qwt", [128, 8, 128])
    d_kw = din("kwt", [128, 2, 8])
    d_cd2 = din("cd2", [128, 8])
    d_mask = din("mask", [128, 2, 128])
    d_invf = din("invf", [128, 96])
    out_d = nc.dram_tensor("out", [16, 128, 1024], F32, kind="ExternalOutput").ap()

    with ExitStack() as es:
        ARENA_BYTES = 207 * 1024
        AR = es.enter_context(nc.sbuf_tensor("arena", [128, ARENA_BYTES // 4], F32))
        PS = [es.enter_context(nc.psum_tensor("psb%d" % i, [128, 512], F32)) for i in range(8)]
        PB = [Buf("ps%d" % i) for i in range(8)]
        S = Sched(nc)
        top = [0]
        uid = [0]

        class T:
            pass

        def alloc(shape, dt, name=None):
            esz = 4 if dt in (F32, I32) else 2
            n = int(np.prod(shape[1:])) * esz
            n = (n + 31) // 32 * 32
            off = top[0]
            top[0] += n
            assert top[0] <= ARENA_BYTES, ("arena overflow", name, top[0])
            v = AR[:shape[0], off // 4:(off + n) // 4]
            nelem = int(np.prod(shape[1:]))
            if dt == BF16:
                v = v.bitcast(BF16)[:, 0:nelem]
            elif dt == I32:
                v = v.bitcast(I32)[:, 0:nelem]
            else:
                v = v[:, 0:nelem]
            if len(shape) == 3:
                v = v.rearrange("p (a b) -> p a b", a=shape[1])
            elif len(shape) == 4:
                v = v.rearrange("p (a b c) -> p a b c", a=shape[1], b=shape[2])
            t = T()
            t.ap = v
            uid[0] += 1
            t.b = Buf((name or "t") + str(uid[0]))
            t.name = t.b.name
            return t

        def MM(out, lhsT, rhs, start, stop, R, W):
            S.add("pe", lambda e: e.matmul(out, lhsT=lhsT, rhs=rhs, start=start, stop=stop), R, W)

        def TR(out, in_, ident, R, W):
            S.add("pe", lambda e: e.transpose(out, in_, ident), R, W)

        def ACT(out, in_, func, R, W, bias=0.0, scale=1.0, accum=None):
            if accum is None:
                S.add("act", lambda e: e.activation(out=out, in_=in_, func=func, bias=bias, scale=scale), R, W)
            else:
                S.add("act", lambda e: e.activation(out=out, in_=in_, func=func, bias=bias, scale=scale, accum_out=accum), R, W)

        def TT(eng, out, in0, in1, op, R, W):
            S.add(eng, lambda e: e.tensor_tensor(out=out, in0=in0, in1=in1, op=op), R, W)

        def TS(eng, out, in0, s1, s2, op0, op1, R, W):
            if s2 is None:
                S.add(eng, lambda e: e.tensor_scalar(out=out, in0=in0, scalar1=s1, scalar2=None, op0=op0), R, W)
            else:
                S.add(eng, lambda e: e.tensor_scalar(out=out, in0=in0, scalar1=s1, scalar2=s2, op0=op0, op1=op1), R, W)

        def STT(eng, out, in0, scalar, in1, op0, op1, R, W):
            S.add(eng, lambda e: e.scalar_tensor_tensor(out=out, in0=in0, scalar=scalar, in1=in1, op0=op0, op1=op1), R, W)

        def CP(eng, out, in_, R, W):
            if eng == "act":
                S.add("act", lambda e: e.copy(out=out, in_=in_), R, W)
            else:
                S.add(eng, lambda e: e.tensor_copy(out=out, in_=in_), R, W)

        def DMA(eng, out, in_, R, W, dsem):
            S.add(eng, lambda e: e.dma_start(out=out, in_=in_), R, W, dsem=dsem)

        def bc(ap, shape, axis):
            return ap.unsqueeze(axis).to_broadcast(list(shape))

        ident_f = alloc([128, 128], F32, "identf")
        ident_b = alloc([128, 128], BF16, "identb")
        ones_b = alloc([128, 128], BF16, "ones")
        S.add("pool", lambda e: e.iota(ident_f.ap, [[1, 128]], base=0, channel_multiplier=-1,
                                       allow_small_or_imprecise_dtypes=True), (), [ident_f.b])
        S.add("dve", lambda e: e.tensor_single_scalar(out=ident_f.ap, in_=ident_f.ap, scalar=0.0, op=ALU.is_equal),
              [ident_f.b], [ident_f.b])
        CP("dve", ident_b.ap, ident_f.ap, [ident_f.b], [ident_b.b])
        S.add("pool", lambda e: e.memset(ones_b.ap, 1.0), (), [ones_b.b])
        mask = alloc([128, 2, 128], F32, "mask")
        DMA("sp", mask.ap, d_mask, (), [mask.b], "d_c0")
        posA_i = alloc([128, 32], I32, "posAi")
        posO_i = alloc([128, 16], I32, "posOi")
        posA = alloc([128, 32], F32, "posA")
        posO = alloc([128, 16], F32, "posO")
        invf = alloc([128, 96], F32, "invf")
        DMA("sp", posA_i.ap, pos_all, (), [posA_i.b], "d_c1")
        DMA("sp", posO_i.ap, pos_own, (), [posO_i.b], "d_c2")
        DMA("sp", invf.ap, d_invf, (), [invf.b], "d_c3")
        CP("dve", posA.ap, posA_i.ap, [posA_i.b], [posA.b])
        CP("dve", posO.ap, posO_i.ap, [posO_i.b], [posO.b])
        latent_off = top[0]
        cqnT = alloc([128, 3, 2048], BF16, "cqnT")
        ckvnT = alloc([128, 2, 4096], BF16, "ckvnT")
        krT2 = alloc([128, 4096], BF16, "krT2")
        qrT = alloc([128, 4, 2048], BF16, "qrT")
        mix_off = top[0]
        mixT = alloc([128, 8, 2048], BF16, "mixT")
        persist_top = top[0]

        def rope_tables(pos_t, cols, f0, nf, scale, temps, cs, sn):
            tt, ti, tf = temps
            n = len(cols)
            tta, tia, tfa = tt.ap[:, 0:n, 0:nf], ti.ap[:, 0:n, 0:nf], tf.ap[:, 0:n, 0:nf]
            for i, bl in enumerate(cols):
                TS("dve", tt.ap[:, i, 0:nf], invf.ap[:, f0:f0 + nf], pos_t.ap[:, bl:bl + 1], None, ALU.mult, None,
                   [invf.b, pos_t.b], [tt.b])
            CP("dve", tia, tta, [tt.b], [ti.b])
            CP("dve", tfa, tia, [ti.b], [tf.b])
            TT("dve", tfa, tta, tfa, ALU.subtract, [tt.b, tf.b], [tf.b])
            ACT(sn.ap, tfa, AF.Sin, [tf.b], [sn.b], scale=6.28318 * 1.0)
            TS("dve", tta, tta, 0.25, None, ALU.add, None, [tt.b], [tt.b])
            CP("dve", tia, tta, [tt.b], [ti.b])
            CP("dve", tfa, tia, [ti.b], [tf.b])
            TT("dve", tfa, tta, tfa, ALU.subtract, [tt.b, tf.b], [tf.b])
            ACT(cs.ap, tfa, AF.Sin, [tf.b], [cs.b], scale=6.28318 * 1.0)
            if scale != 1.0:
                TS("dve", cs.ap, cs.ap, scale, None, ALU.mult, None, [cs.b], [cs.b])
                TS("dve", sn.ap, sn.ap, scale, None, ALU.mult, None, [sn.b], [sn.b])

        def rotary(src, srcb, dst, dstb, cs, sn, csb, nh, half, tmp):
            x1 = src[:, :, 0:half]
            x2 = src[:, :, half:2 * half]
            cb = bc(cs, [128, nh, half], 1)
            sb_ = bc(sn, [128, nh, half], 1)
            t1, t2, t3, t4 = tmp
            a1 = t1.ap[:, 0:nh, 0:half]
            a2 = t2.ap[:, 0:nh, 0:half]
            a3 = t3.ap[:, 0:nh, 0:half]
            a4 = t4.ap[:, 0:nh, 0:half]
            TT("dve", a1, x1, cb, ALU.mult, srcb + csb, [t1.b])
            TT("dve", a2, x2, sb_, ALU.mult, srcb + csb, [t2.b])
            TT("dve", a3, x2, cb, ALU.mult, srcb + csb, [t3.b])
            TT("dve", a4, x1, sb_, ALU.mult, srcb + csb, [t4.b])
            TT("pool", dst[:, :, 0:half], a1, a2, ALU.subtract, [t1.b, t2.b], dstb)
            TT("pool", dst[:, :, half:2 * half], a3, a4, ALU.add, [t3.b, t4.b], dstb)

        def rms_scale(psrc, psb, nm, ntok, dst, dstb, inv_n, eps, sq, ssq_ps, ssq_pb, rr):
            ACT(sq.ap[:, 0:nm, 0:ntok], psrc, AF.Square, psb, [sq.b])
            for m in range(nm):
                MM(ssq_ps[:, 0:ntok], ones_b.ap, sq.ap[:, m, 0:ntok], m == 0, m == nm - 1, [ones_b.b, sq.b], [ssq_pb])
            ACT(rr.ap[:, 0:ntok], ssq_ps[:, 0:ntok], AF.Sqrt, [ssq_pb], [rr.b], bias=eps, scale=inv_n)
            S.add("dve", lambda e: e.reciprocal(out=rr.ap[:, 0:ntok], in_=rr.ap[:, 0:ntok]), [rr.b], [rr.b])
            TT("dve", dst, psrc, bc(rr.ap[:, 0:ntok], [128, nm, ntok], 1), ALU.mult, psb + [rr.b], dstb)

        wcq = alloc([128, 8, 384], BF16, "wcq")
        wckv = alloc([128, 8, 320], BF16, "wckv")
        wuq_f = alloc([128, 3, 1536], F32, "wuqf")
        wuq = alloc([128, 3, 1536], BF16, "wuq")
        qg = alloc([128, 3], F32, "qg")
        DMA("pool", wcq.ap, d_wcq, (), [wcq.b], "d_w0")
        DMA("pool", wckv.ap, d_wckv, (), [wckv.b], "d_w1")
        DMA("sp", wuq_f.ap, d_wuq, (), [wuq_f.b], "d_w2")
        DMA("sp", qg.ap, d_qg, (), [qg.b], "d_c4")
        for m in range(3):
            TS("dve", wuq.ap[:, m, :], wuq_f.ap[:, m, :], qg.ap[:, m:m + 1], None, ALU.mult, None,
               [wuq_f.b, qg.b], [wuq.b])
        rtmp = (alloc([128, 32, 32], F32, "rtt"), alloc([128, 32, 32], I32, "rti"), alloc([128, 32, 32], F32, "rtf"))
        cMo, sMo = alloc([128, 16, 32], F32, "cMo"), alloc([128, 16, 32], F32, "sMo")
        cMa, sMa = alloc([128, 32, 32], F32, "cMa"), alloc([128, 32, 32], F32, "sMa")
        rope_tables(posO, list(range(16)), 64, 32, SM_SCALE, rtmp, cMo, sMo)
        rope_tables(posA, list(range(32)), 64, 32, 1.0, rtmp, cMa, sMa)
        xa = [alloc([128, 8, 256], BF16, "xa") for _ in range(2)]
        xo = [alloc([128, 8, 128], BF16, "xo") for _ in range(2)]
        sq = alloc([128, 3, 256], BF16, "sq")
        rr = alloc([128, 256], F32, "rr")
        qr_tok = alloc([128, 8, 64], BF16, "qrtok")
        kr_tok = alloc([128, 2, 64], BF16, "krtok")
        tmp1 = tuple(alloc([128, 8, 32], F32, "t%d" % i) for i in range(4))
        for j in range(NPAIR):
            A = xa[j % 2]
            O = xo[j % 2]
            if j == 0:
                DMA("pool", A.ap, xT_all[:, :, 0:256], (), [A.b], "d_xa0")
                DMA("pool", O.ap, xT_own[:, :, 0:128], (), [O.b], "d_xo0")
            if j + 1 < NPAIR:
                An, On = xa[(j + 1) % 2], xo[(j + 1) % 2]
                DMA("pool", An.ap, xT_all[:, :, (j + 1) * 256:(j + 2) * 256], (), [An.b], "d_xa%d" % ((j + 1) % 2))
                DMA("pool", On.ap, xT_own[:, :, (j + 1) * 128:(j + 2) * 128], (), [On.b], "d_xo%d" % ((j + 1) % 2))
            pq = PS[0][:, 0:384].rearrange("p (m t) -> p m t", m=3)
            for m in range(3):
                for k in range(8):
                    MM(pq[:, m, :], wcq.ap[:, k, m * 128:(m + 1) * 128], O.ap[:, k, :], k == 0, k == 7,
                       [wcq.b, O.b], [PB[0]])
            rms_scale(pq, [PB[0]], 3, 128, cqnT.ap[:, :, j * 128:(j + 1) * 128], [cqnT.b], 1.0 / 384, 1e-6,
                      sq, PS[1], PB[1], rr)
            for m in range(3):
                MM(PS[2][:, :], cqnT.ap[:, m, j * 128:(j + 1) * 128], wuq.ap[:, m, 1024:1536], m == 0, m == 2,
                   [cqnT.b, wuq.b], [PB[2]])
            rotary(PS[2][:, :].rearrange("p (h d) -> p h d", h=8), [PB[2]], qr_tok.ap, [qr_tok.b],
                   cMo.ap[:, j, :], sMo.ap[:, j, :], [cMo.b, sMo.b], 8, 32, tmp1)
            pt = PS[3][:, 0:256].bitcast(BF16).rearrange("p (a t) -> p a t", a=4)
            for hp in range(4):
                TR(pt[:, hp, :], qr_tok.ap[:, 2 * hp:2 * hp + 2, :].rearrange("p a d -> p (a d)"), ident_b.ap,
                   [qr_tok.b, ident_b.b], [PB[3]])
            CP("act", qrT.ap[:, :, j * 128:(j + 1) * 128], pt, [PB[3]], [qrT.b])
            pk = PS[4][:, :].rearrange("p (m t) -> p m t", m=2)
            for m in range(2):
                for k in range(8):
                    MM(pk[:, m, :], wckv.ap[:, k, m * 128:(m + 1) * 128], A.ap[:, k, :], k == 0, k == 7,
                       [wckv.b, A.b], [PB[4]])
            rms_scale(pk, [PB[4]], 2, 256, ckvnT.ap[:, :, j * 256:(j + 1) * 256], [ckvnT.b], 1.0 / 256, 1e-6,
                      sq, PS[5], PB[5], rr)
            for X in range(2):
                blk = 2 * j + X
                for k in range(8):
                    MM(PS[6][:, 0:64], A.ap[:, k, X * 128:(X + 1) * 128], wckv.ap[:, k, 256:320], k == 0, k == 7,
                       [A.b, wckv.b], [PB[6]])
                rotary(PS[6][:, 0:64].rearrange("p (h d) -> p h d", h=1), [PB[6]], kr_tok.ap[:, 0:1, :], [kr_tok.b],
                       cMa.ap[:, blk, :], sMa.ap[:, blk, :], [cMa.b, sMa.b], 1, 32, tmp1)
                CP("pool", kr_tok.ap[:, 1, :], kr_tok.ap[:, 0, :], [kr_tok.b], [kr_tok.b])
                pt2 = PS[7][:, 0:64].bitcast(BF16)
                TR(pt2, kr_tok.ap.rearrange("p a d -> p (a d)"), ident_b.ap, [kr_tok.b, ident_b.b], [PB[7]])
                CP("act", krT2.ap[:, blk * 128:(blk + 1) * 128], pt2, [PB[7]], [krT2.b])

        S.barrier()
        top[0] = persist_top
        wrq = alloc([128, 8, 1024], BF16, "wrq")
        wrk = alloc([128, 8, 1024], BF16, "wrk")
        wrv = alloc([128, 8, 1024], BF16, "wrv")
        for i, (w, d) in enumerate(((wrq, d_wrq), (wrk, d_wrk), (wrv, d_wrv))):
            for hf in range(2):
                DMA("pool", w.ap[:, :, hf * 512:(hf + 1) * 512], d[:, :, hf * 512:(hf + 1) * 512], (), [w.b], "d_w%d" % (3 + i))
        tab = alloc([128, 2, 8, 128], F32, "tab")
        qwt = alloc([128, 8, 128], F32, "qwt")
        kwt = alloc([128, 2, 8], F32, "kwt")
        cd2 = alloc([128, 8], F32, "cd2")
        DMA("sp", tab.ap, d_tab, (), [tab.b], "d_c5")
        DMA("sp", qwt.ap, d_qw, (), [qwt.b], "d_c6")
        DMA("sp", kwt.ap, d_kw, (), [kwt.b], "d_c7")
        DMA("sp", cd2.ap, d_cd2, (), [cd2.b], "d_c8")
        rtmp = (alloc([128, 2, 64], F32, "rtt"), alloc([128, 2, 64], I32, "rti"), alloc([128, 2, 64], F32, "rtf"))
        cRo, sRo = alloc([128, 1, 64], F32, "cRo"), alloc([128, 1, 64], F32, "sRo")
        cRa, sRa = alloc([128, 2, 64], F32, "cRa"), alloc([128, 2, 64], F32, "sRa")
        xa = [alloc([128, 8, 256], BF16, "xa") for _ in range(2)]
        xo = [alloc([128, 8, 128], BF16, "xo") for _ in range(2)]
        tmp2 = tuple(alloc([128, 4, 64], F32, "t%d" % i) for i in range(4))
        k_tok = alloc([128, 8, 128], BF16, "ktok")
        kw_tok = alloc([128, 2, 8, 128], BF16, "kwtok")
        v_tok = alloc([128, 2, 8, 128], BF16, "vtok")
        q_tok = alloc([128, 8, 128], BF16, "qtok")
        kT = alloc([128, 2, 8, 128], BF16, "kT")
        qT = alloc([128, 8, 128], BF16, "qT")
        qwT = alloc([128, 8, 128], BF16, "qwT")
        sc = alloc([128, 8, 2, 128], BF16, "sc")
        Rf = alloc([128, 8, 128], F32, "Rf")
        Rb = alloc([128, 8, 128], BF16, "Rb")
        o_sb = alloc([128, 8, 128], F32, "osb")
        o_sq = alloc([128, 8, 128], BF16, "osq")
        gn_b = alloc([128, 8, 128], BF16, "gnb")
        st = alloc([128, 32], F32, "st")
        S.add("pool", lambda e: e.memset(Rf.ap, 0.0), (), [Rf.b])
        S.add("pool", lambda e: e.memset(Rb.ap, 0.0), (), [Rb.b])

        def proj_tok(xsrc, xb, w, pa, pb_):
            for hf in range(2):
                for k in range(8):
                    MM(PS[pa + hf][:, :], xsrc[:, k, :], w.ap[:, k, hf * 512:(hf + 1) * 512], k == 0, k == 7,
                       [xb, w.b], [PB[pa + hf]])

        def ps2(pa):
            return [PS[pa + hf][:, :].rearrange("p (h d) -> p h d", h=4) for hf in range(2)]

        for j in range(NPAIR):
            A = xa[j % 2]
            O = xo[j % 2]
            if j == 0:
                DMA("pool", A.ap, xT_all[:, :, 0:256], (), [A.b], "d_xa0")
                DMA("pool", O.ap, xT_own[:, :, 0:128], (), [O.b], "d_xo0")
            if j + 1 < NPAIR:
                An, On = xa[(j + 1) % 2], xo[(j + 1) % 2]
                DMA("pool", An.ap, xT_all[:, :, (j + 1) * 256:(j + 2) * 256], (), [An.b], "d_xa%d" % ((j + 1) % 2))
                DMA("pool", On.ap, xT_own[:, :, (j + 1) * 128:(j + 2) * 128], (), [On.b], "d_xo%d" % ((j + 1) % 2))
            rope_tables(posA, [2 * j, 2 * j + 1], 0, 64, 1.0, rtmp, cRa, sRa)
            rope_tables(posO, [j], 0, 64, 1.0, rtmp, cRo, sRo)
            for X in range(2):
                blk = 2 * j + X
                xs = A.ap[:, :, X * 128:(X + 1) * 128]
                proj_tok(xs, A.b, wrk, 0, None)
                pv = ps2(0)
                for hf in range(2):
                    rotary(pv[hf], [PB[hf]], k_tok.ap[:, hf * 4:(hf + 1) * 4, :], [k_tok.b],
                           cRa.ap[:, X, :], sRa.ap[:, X, :], [cRa.b, sRa.b], 4, 64, tmp2)
                TT("dve", kw_tok.ap[:, X, :, :], k_tok.ap, bc(kwt.ap[:, X, :], [128, 8, 128], 2), ALU.mult,
                   [k_tok.b, kwt.b], [kw_tok.b])
                ptk = PS[4][:, :].bitcast(BF16).rearrange("p (h t) -> p h t", h=8)
                for h in range(8):
                    TR(ptk[:, h, :], k_tok.ap[:, h, :], ident_b.ap, [k_tok.b, ident_b.b], [PB[4]])
                CP("act", kT.ap[:, X, :, :], ptk, [PB[4]], [kT.b])
                proj_tok(xs, A.b, wrv, 2, None)
                pv = ps2(2)
                for hf in range(2):
                    CP("act", v_tok.ap[:, X, hf * 4:(hf + 1) * 4, :], pv[hf], [PB[2 + hf]], [v_tok.b])
            proj_tok(O.ap, O.b, wrq, 0, None)
            pv = ps2(0)
            for hf in range(2):
                rotary(pv[hf], [PB[hf]], q_tok.ap[:, hf * 4:(hf + 1) * 4, :], [q_tok.b],
                       cRo.ap[:, 0, :], sRo.ap[:, 0, :], [cRo.b, sRo.b], 4, 64, tmp2)
            ptq = PS[4][:, :].bitcast(BF16).rearrange("p (h t) -> p h t", h=8)
            for h in range(8):
                TR(ptq[:, h, :], q_tok.ap[:, h, :], ident_b.ap, [q_tok.b, ident_b.b], [PB[4]])
            CP("act", qT.ap, ptq, [PB[4]], [qT.b])
            TT("dve", qwT.ap, qT.ap, qwt.ap, ALU.mult, [qT.b, qwt.b], [qwT.b])
            for h2 in range(4):
                pss = PS[5][:, :].rearrange("p (h x q) -> p h x q", h=2, x=2)
                for hh in range(2):
                    h = 2 * h2 + hh
                    for X in range(2):
                        MM(pss[:, hh, X, :], kT.ap[:, X, h, :], qT.ap[:, h, :], True, True, [kT.b, qT.b], [PB[5]])
                TT("dve", sc.ap[:, 2 * h2:2 * h2 + 2, :, :], pss,
                   tab.ap[:, :, 2 * h2:2 * h2 + 2, :].rearrange("p x h q -> p h x q"), ALU.mult,
                   [PB[5], tab.b], [sc.b])
            po = [PS[6][:, :].rearrange("p (h d) -> p h d", h=4), PS[7][:, :].rearrange("p (h d) -> p h d", h=4)]
            for h in range(8):
                o_ps = po[h // 4][:, h % 4, :]
                pbb = PB[6 + h // 4]
                MM(o_ps, sc.ap[:, h, 0, :], v_tok.ap[:, 0, h, :], True, False, [sc.b, v_tok.b], [pbb])
                MM(o_ps, sc.ap[:, h, 1, :], v_tok.ap[:, 1, h, :], False, False, [sc.b, v_tok.b], [pbb])
                MM(o_ps, qwT.ap[:, h, :], Rb.ap[:, h, :], False, True, [qwT.b, Rb.b], [pbb])
            for hf in range(2):
                CP("act", o_sb.ap[:, hf * 4:(hf + 1) * 4, :], po[hf], [PB[6 + hf]], [o_sb.b])
            S.add("dve", lambda e: e.tensor_reduce(out=st.ap[:, 0:8], in_=o_sb.ap, axis=AX.X, op=ALU.add), [o_sb.b], [st.b])
            TS("dve", st.ap[:, 0:8], st.ap[:, 0:8], 1.0 / 128, None, ALU.mult, None, [st.b], [st.b])
            TT("dve", o_sb.ap, o_sb.ap, bc(st.ap[:, 0:8], [128, 8, 128], 2), ALU.subtract, [o_sb.b, st.b], [o_sb.b])
            TT("pool", o_sq.ap, o_sb.ap, o_sb.ap, ALU.mult, [o_sb.b], [o_sq.b])
            S.add("dve", lambda e: e.tensor_reduce(out=st.ap[:, 8:16], in_=o_sq.ap, axis=AX.X, op=ALU.add), [o_sq.b], [st.b])
            ACT(st.ap[:, 16:24], st.ap[:, 8:16], AF.Sqrt, [st.b], [st.b], bias=1e-6, scale=1.0 / 128)
            S.add("dve", lambda e: e.reciprocal(out=st.ap[:, 16:24], in_=st.ap[:, 16:24]), [st.b], [st.b])
            TT("dve", gn_b.ap, o_sb.ap, bc(st.ap[:, 16:24], [128, 8, 128], 2), ALU.mult, [o_sb.b, st.b], [gn_b.b])
            ptg = PS[4][:, :].bitcast(BF16).rearrange("p (h t) -> p h t", h=8)
            for h in range(8):
                TR(ptg[:, h, :], gn_b.ap[:, h, :], ident_b.ap, [gn_b.b, ident_b.b], [PB[4]])
            CP("act", mixT.ap[:, :, j * 128:(j + 1) * 128], ptg, [PB[4]], [mixT.b])
            for h in range(8):
                u_ps = po[h // 4][:, h % 4, :]
                pbb = PB[6 + h // 4]
                MM(u_ps, kw_tok.ap[:, 0, h, :], v_tok.ap[:, 0, h, :], True, False, [kw_tok.b, v_tok.b], [pbb])
                MM(u_ps, kw_tok.ap[:, 1, h, :], v_tok.ap[:, 1, h, :], False, True, [kw_tok.b, v_tok.b], [pbb])
            TT("dve", Rf.ap, Rf.ap, bc(cd2.ap, [128, 8, 128], 2), ALU.mult, [Rf.b, cd2.b], [Rf.b])
            for hf in range(2):
                TT("dve", Rf.ap[:, hf * 4:(hf + 1) * 4, :], Rf.ap[:, hf * 4:(hf + 1) * 4, :], po[hf], ALU.add,
                   [Rf.b, PB[6 + hf]], [Rf.b])
            CP("act", Rb.ap, Rf.ap, [Rf.b], [Rb.b])

        S.barrier()
        top[0] = persist_top
        xTo = alloc([128, 8, 2048], BF16, "xTo")
        for q4 in range(4):
            DMA("pool", xTo.ap[:, :, q4 * 512:(q4 + 1) * 512], xT_own[:, :, q4 * 512:(q4 + 1) * 512], (), [xTo.b], "d_xto")
        off_stage = top[0]
        wukv_f = alloc([128, 2, 2048], F32, "wukvf")
        wukv = alloc([128, 2, 2048], BF16, "wukv")
        kvg = alloc([128, 2], F32, "kvg")
        DMA("sp", wukv_f.ap, d_wukv, (), [wukv_f.b], "d_w6")
        DMA("sp", kvg.ap, d_kvg, (), [kvg.b], "d_c9")
        for m in range(2):
            TS("dve", wukv.ap[:, m, :], wukv_f.ap[:, m, :], kvg.ap[:, m:m + 1], None, ALU.mult, None,
               [wukv_f.b, kvg.b], [wukv.b])
        wuq_f = alloc([128, 3, 1024], F32, "wuqf")
        wuqn = alloc([128, 3, 1024], BF16, "wuqn")
        qg3 = alloc([128, 3], F32, "qg3")
        DMA("sp", wuq_f.ap, d_wuq[:, :, 0:1024], (), [wuq_f.b], "d_w2")
        DMA("sp", qg3.ap, d_qg, (), [qg3.b], "d_c4")
        for m in range(3):
            TS("dve", wuqn.ap[:, m, :], wuq_f.ap[:, m, :], qg3.ap[:, m:m + 1], None, ALU.mult, None,
               [wuq_f.b, qg3.b], [wuqn.b])
        wg = [alloc([128, 8, 384], BF16, "wg") for _ in range(2)]
        save_top = top[0]
        top[0] = off_stage
        knT = alloc([128, 4096], BF16, "knT")
        Vh = alloc([128, 32, 128], BF16, "Vh")
        assert top[0] <= off_stage + 16 * 1024

        def inherit(t_, src_):
            for k_, v_ in src_.b.r.items():
                if k_ not in t_.b.r or t_.b.r[k_][0] < v_[0]:
                    t_.b.r[k_] = v_
            for k_, v_ in src_.b.w.items():
                if k_ not in t_.b.w or t_.b.w[k_][0] < v_[0]:
                    t_.b.w[k_] = v_

        inherit(knT, wukv_f)
        inherit(Vh, wukv_f)
        top[0] = save_top
        qnT = alloc([128, 2048], BF16, "qnT")
        qrz = [alloc([128, 2048], BF16, "qrz") for _ in range(2)]
        S.add("pool", lambda e: e.memset(qrz[0].ap[64:128, :], 0.0), (), [qrz[0].b])
        S.add("pool", lambda e: e.memset(qrz[1].ap[0:64, :], 0.0), (), [qrz[1].b])
        pT = [alloc([128, 512], BF16, "pT") for _ in range(2)]
        rD = alloc([128, 512], F32, "rD")
        o_f = alloc([128, 512], F32, "of")
        g_mla = alloc([128, 512], F32, "gmla")
        g_ret = alloc([128, 512], F32, "gret")
        g_sil = alloc([128, 512], F32, "gsil")
        cnt = [0]
        for h in range(8):
            W = wg[h % 2]
            DMA("pool", W.ap, d_wgate[h], (), [W.b], "d_wg%d" % (h % 2))
            hp = (h % 2) * 64
            QZ = qrz[h % 2]
            CP("pool", QZ.ap[hp:hp + 64, :], qrT.ap[hp:hp + 64, h // 2, :], [qrT.b], [QZ.b])
            for tg in range(8):
                pb = tg % 2
                for m in range(2):
                    MM(PS[pb][:, :], wukv.ap[:, m, h * 256:h * 256 + 128], ckvnT.ap[:, m, tg * 512:(tg + 1) * 512],
                       m == 0, m == 1, [wukv.b, ckvnT.b], [PB[pb]])
                CP("act" if tg % 2 == 0 else "dve", knT.ap[:, tg * 512:(tg + 1) * 512], PS[pb][:, :], [PB[pb]], [knT.b])
            for b4 in range(8):
                pb = b4 % 2
                pvv = PS[pb][:, :].rearrange("p (a d) -> p a d", a=4)
                for a in range(4):
                    blk = b4 * 4 + a
                    for m in range(2):
                        MM(pvv[:, a, :], ckvnT.ap[:, m, blk * 128:(blk + 1) * 128],
                           wukv.ap[:, m, h * 256 + 128:h * 256 + 256], m == 0, m == 1, [ckvnT.b, wukv.b], [PB[pb]])
                CP("dve" if b4 % 2 == 0 else "act", Vh.ap[:, b4 * 4:(b4 + 1) * 4, :], pvv, [PB[pb]], [Vh.b])
            for g in range(4):
                pb = g % 2
                for m in range(3):
                    MM(PS[pb][:, :], wuqn.ap[:, m, h * 128:(h + 1) * 128], cqnT.ap[:, m, g * 512:(g + 1) * 512],
                       m == 0, m == 2, [wuqn.b, cqnT.b], [PB[pb]])
                ACT(qnT.ap[:, g * 512:(g + 1) * 512], PS[pb][:, :], AF.Copy, [PB[pb]], [qnT.b], scale=SM_SCALE)
            for g in range(4):
                gs = slice(g * 512, (g + 1) * 512)
                for t3, (gt, fn) in enumerate(((g_mla, AF.Sigmoid), (g_ret, AF.Sigmoid), (g_sil, AF.Silu))):
                    pb = 2 + (t3 % 2)
                    for k in range(8):
                        MM(PS[pb][:, :], W.ap[:, k, t3 * 128:(t3 + 1) * 128], xTo.ap[:, k, gs], k == 0, k == 7,
                           [W.b, xTo.b], [PB[pb]])
                    ACT(gt.ap, PS[pb][:, :], fn, [PB[pb]], [gt.b])
                TT("pool", g_ret.ap, g_ret.ap, g_sil.ap, ALU.mult, [g_ret.b, g_sil.b], [g_ret.b])
                TT("pool", g_ret.ap, g_ret.ap, mixT.ap[:, h, gs], ALU.mult, [g_ret.b, mixT.b], [g_ret.b])
                nkb = 8 * g + 8

                def kb_info(kb):
                    if kb < 8 * g:
                        return 0, None
                    ip = (kb - 8 * g) // 2
                    return ip * 128, (kb - 8 * g) % 2

                def s_stage(kb, slot):
                    c0, diag = kb_info(kb)
                    cs_ = slice(c0, 512)
                    qs_ = slice(g * 512 + c0, (g + 1) * 512)
                    sb_i = 4 + slot
                    MM(PS[sb_i][:, cs_], knT.ap[:, kb * 128:(kb + 1) * 128], qnT.ap[:, qs_], True, False,
                       [knT.b, qnT.b], [PB[sb_i]])
                    MM(PS[sb_i][:, cs_], krT2.ap[:, kb * 128:(kb + 1) * 128], QZ.ap[:, qs_],
                       False, True, [krT2.b, QZ.b], [PB[sb_i]])

                def e_stage(kb, slot):
                    c0, diag = kb_info(kb)
                    cs_ = slice(c0, 512)
                    sb_i = 4 + slot
                    P = pT[slot]
                    ACT(P.ap[:, cs_], PS[sb_i][:, cs_], AF.Exp, [PB[sb_i]], [P.b])
                    if diag is not None:
                        TT("dve", P.ap[:, c0:c0 + 128], P.ap[:, c0:c0 + 128], mask.ap[:, diag, :], ALU.mult,
                           [P.b, mask.b], [P.b])

                def pv_stage(kb, slot):
                    c0, diag = kb_info(kb)
                    cs_ = slice(c0, 512)
                    P = pT[slot]
                    MM(PS[6][:, cs_], Vh.ap[:, kb, :], P.ap[:, cs_], kb == 0, kb == nkb - 1, [Vh.b, P.b], [PB[6]])
                    MM(PS[7][:, cs_], ones_b.ap, P.ap[:, cs_], kb == 0, kb == nkb - 1, [ones_b.b, P.b], [PB[7]])

                base = cnt[0]
                cnt[0] += nkb
                s_stage(0, base % 2)
                for kb in range(nkb):
                    if kb + 1 < nkb:
                        s_stage(kb + 1, (base + kb + 1) % 2)
                    e_stage(kb, (base + kb) % 2)
                    pv_stage(kb, (base + kb) % 2)
                S.add("dve", lambda e: e.reciprocal(out=rD.ap, in_=PS[7][:, :]), [PB[7]], [rD.b])
                TT("dve", o_f.ap, PS[6][:, :], rD.ap, ALU.mult, [PB[6], rD.b], [o_f.b])
                TT("pool", o_f.ap, o_f.ap, g_mla.ap, ALU.mult, [o_f.b, g_mla.b], [o_f.b])
                TT("pool", mixT.ap[:, h, gs], o_f.ap, g_ret.ap, ALU.add, [o_f.b, g_ret.b], [mixT.b])

        S.barrier()
        top[0] = persist_top
        p4_top = top[0]
        top[0] = latent_off
        h1b = alloc([128, 16, 1024], BF16, "h1b")
        G = alloc([128, 16, 32], F32, "G")
        G17 = alloc([128, 32], F32, "G17")
        Ghl = alloc([128, 16, 32, 2], BF16, "Ghl")
        RK = alloc([128, 16, 32], F32, "RK")
        MKb = alloc([128, 16, 32], BF16, "MKb")
        Lst = alloc([128, 128], BF16, "Lst")
        iotaC = alloc([128, 128], F32, "iotaC")
        assert top[0] <= mix_off
        top[0] = p4_top
        acc = alloc([128, 16, 1024], F32, "acc")
        sm = alloc([128, 8], F32, "sm")
        junk = alloc([128, 1024], BF16, "junk")
        p5_keep = top[0]
        lnp = alloc([128, 2, 1024], F32, "lnp")
        for i in range(2):
            DMA("sp", lnp.ap[:, i, :], d_ln[i:i + 1, :].partition_broadcast(128), (), [lnp.b], "d_ln")
        Lf = alloc([128, 384], F32, "Lf")
        S.add("pool", lambda e: e.iota(Lf.ap[:, 0:128], [[1, 128]], base=0, channel_multiplier=-1,
                                       allow_small_or_imprecise_dtypes=True), (), [Lf.b])
        S.add("dve", lambda e: e.tensor_single_scalar(out=Lst.ap, in_=Lf.ap[:, 0:128], scalar=0.0, op=ALU.is_gt),
              [Lf.b], [Lst.b])
        S.add("pool", lambda e: e.iota(iotaC.ap, [[1, 128]], base=0, channel_multiplier=0,
                                       allow_small_or_imprecise_dtypes=True), (), [iotaC.b])
        wo = alloc([128, 8, 1024], BF16, "wo")
        for hf in range(2):
            DMA("pool", wo.ap[:, :, hf * 512:(hf + 1) * 512], d_wo[:, :, hf * 512:(hf + 1) * 512], (), [wo.b], "d_wo")
        wr = alloc([128, 8, 32], F32, "wr")
        brr = alloc([128, 32], F32, "brr")
        bdn = alloc([32, 1024], F32, "bdn")
        DMA("sp", wr.ap, d_wr, (), [wr.b], "d_c10")
        DMA("sp", brr.ap, d_br.partition_broadcast(128), (), [brr.b], "d_c11")
        DMA("sp", bdn.ap, d_bdn, (), [bdn.b], "d_c12")
        xres = [alloc([128, 1024], F32, "xres") for _ in range(2)]
        vv = alloc([128, 1024], F32, "vv")
        h1 = alloc([128, 1024], F32, "h1")
        h1Tf = alloc([128, 8, 128], F32, "h1Tf")
        lg = alloc([128, 32], F32, "lg")
        em = alloc([128, 32], F32, "em")
        mk = alloc([128, 32], F32, "mk")
        t8 = alloc([128, 8], F32, "t8")
        GT = alloc([32, 128], F32, "GT")

        def layernorm(src, srcb, dst, dstb, gi, bi):
            S.add("dve", lambda e: e.tensor_reduce(out=sm.ap[:, 0:1], in_=src, axis=AX.X, op=ALU.add), srcb, [sm.b])
            TS("dve", sm.ap[:, 0:1], sm.ap[:, 0:1], -1.0 / 1024, None, ALU.mult, None, [sm.b], [sm.b])
            ACT(src, src, AF.Identity, srcb + [sm.b], srcb, bias=sm.ap[:, 0:1])
            ACT(junk.ap, src, AF.Square, srcb, [junk.b, sm.b], accum=sm.ap[:, 1:2])
            ACT(sm.ap[:, 2:3], sm.ap[:, 1:2], AF.Sqrt, [sm.b], [sm.b], bias=1e-5, scale=1.0 / 1024)
            S.add("dve", lambda e: e.reciprocal(out=sm.ap[:, 2:3], in_=sm.ap[:, 2:3]), [sm.b], [sm.b])
            STT("dve", dst, src, sm.ap[:, 2:3], lnp.ap[:, gi, :], ALU.mult, ALU.mult, srcb + [sm.b, lnp.b], dstb)
            TT("pool", dst, dst, lnp.ap[:, bi, :], ALU.add, dstb + [lnp.b], dstb)

        for j in range(16):
            XR = xres[j % 2]
            DMA("sp", XR.ap, x_own[j], (), [XR.b], "d_xr%d" % (j % 2))
            for hf in range(2):
                for c in range(8):
                    MM(PS[hf][:, :], mixT.ap[:, c, j * 128:(j + 1) * 128], wo.ap[:, c, hf * 512:(hf + 1) * 512],
                       c == 0, c == 7, [mixT.b, wo.b], [PB[hf]])
            for hf in range(2):
                STT("dve", vv.ap[:, hf * 512:(hf + 1) * 512], XR.ap[:, hf * 512:(hf + 1) * 512], DN_ALPHA, PS[hf][:, :],
                    ALU.mult, ALU.add, [XR.b, PB[hf]], [vv.b])
            S.add("act", lambda e: e.memzero(sm.ap), [sm.b], [sm.b])
            layernorm(vv.ap, [vv.b], h1.ap, [h1.b], 0, 1)
            for c4 in range(2):
                ptf = PS[2 + c4][:, :].rearrange("p (a t) -> p a t", a=4)
                for a in range(4):
                    c = c4 * 4 + a
                    TR(ptf[:, a, :], h1.ap[:, c * 128:(c + 1) * 128], ident_f.ap, [h1.b, ident_f.b], [PB[2 + c4]])
                CP("act", h1Tf.ap[:, c4 * 4:(c4 + 1) * 4, :], ptf, [PB[2 + c4]], [h1Tf.b])
            CP("pool", h1b.ap[:, j, :], h1.ap, [h1.b], [h1b.b])
            for c in range(8):
                MM(PS[4][:, 0:32], h1Tf.ap[:, c, :], wr.ap[:, c, :], c == 0, c == 7, [h1Tf.b, wr.b], [PB[4]])
            TT("dve", lg.ap, PS[4][:, 0:32], brr.ap, ALU.add, [PB[4], brr.b], [lg.b])
            S.add("dve", lambda e: e.max(out=t8.ap, in_=lg.ap), [lg.b], [t8.b])
            TS("dve", mk.ap, lg.ap, t8.ap[:, 3:4], None, ALU.is_ge, None, [lg.b, t8.b], [mk.b])
            TS("dve", sm.ap[:, 4:5], t8.ap[:, 0:1], -1.0, None, ALU.mult, None, [t8.b], [sm.b])
            ACT(em.ap, lg.ap, AF.Exp, [lg.b, sm.b], [em.b], bias=sm.ap[:, 4:5])
            TT("dve", em.ap, em.ap, mk.ap, ALU.mult, [em.b, mk.b], [em.b])
            S.add("dve", lambda e: e.tensor_reduce(out=sm.ap[:, 5:6], in_=em.ap, axis=AX.X, op=ALU.add), [em.b], [sm.b])
            S.add("dve", lambda e: e.reciprocal(out=sm.ap[:, 5:6], in_=sm.ap[:, 5:6]), [sm.b], [sm.b])
            TS("dve", G.ap[:, j, :], em.ap, sm.ap[:, 5:6], None, ALU.mult, None, [em.b, sm.b], [G.b])
            TS("dve", G17.ap, em.ap, sm.ap[:, 5:6], 1.0 / 1.702, ALU.mult, ALU.mult, [em.b, sm.b], [G17.b])
            CP("dve", MKb.ap[:, j, :], mk.ap, [mk.b], [MKb.b])
            CP("dve", Ghl.ap[:, j, :, 0], G17.ap, [G17.b], [Ghl.b])
            TT("dve", G17.ap, G17.ap, Ghl.ap[:, j, :, 0], ALU.subtract, [G17.b, Ghl.b], [G17.b])
            CP("dve", Ghl.ap[:, j, :, 1], G17.ap, [G17.b], [Ghl.b])
            TR(PS[5][0:32, 0:128], G.ap[:, j, :], ident_f.ap, [G.b, ident_f.b], [PB[5]])
            CP("act", GT.ap, PS[5][0:32, 0:128], [PB[5]], [GT.b])
            for hf in range(2):
                MM(PS[6 + hf][:, :], GT.ap, bdn.ap[:, hf * 512:(hf + 1) * 512], True, True, [GT.b, bdn.b], [PB[6 + hf]])
                STT("dve", acc.ap[:, j, hf * 512:(hf + 1) * 512], h1.ap[:, hf * 512:(hf + 1) * 512], DN_ALPHA,
                    PS[6 + hf][:, :], ALU.mult, ALU.add, [h1.b, PB[6 + hf]], [acc.b])

        prk = PS[0][:, :].rearrange("p (j e) -> p j e", j=16)
        for j in range(16):
            nprev = j % 4
            MM(prk[:, j, :], Lst.ap, MKb.ap[:, j, :], True, nprev == 0, [Lst.b, MKb.b], [PB[0]])
            for i2 in range(nprev):
                j2 = (j // 4) * 4 + i2
                MM(prk[:, j, :], ones_b.ap, MKb.ap[:, j2, :], False, i2 == nprev - 1, [ones_b.b, MKb.b], [PB[0]])
        CP("dve", RK.ap, prk, [PB[0]], [RK.b])
        S.barrier()
        C = 512
        top[0] = mix_off
        Pm = alloc([128, 16, 128], BF16, "Pm")
        XeT = alloc([128, 8, C], BF16, "XeT")
        PT = alloc([128, 4, 512], BF16, "PT")
        aT = alloc([128, 8, C], BF16, "aT")
        Ye = alloc([128, 4, 1024], BF16, "Ye")
        assert top[0] <= persist_top
        top[0] = p5_keep
        bup = alloc([128, 32, 16], F32, "bup")
        DMA("sp", bup.ap, d_bup, (), [bup.b], "d_c13")
        bl1 = bup.ap.rearrange("p e (q s) -> p e q s", s=4)[:, :, :, 2:4]
        TS("dve", bl1, bl1, 1.0, None, ALU.add, None, [bup.b], [bup.b])
        wu = [alloc([128, 8, 512], BF16, "wu") for _ in range(2)]
        wd = [alloc([128, 2, 1024], BF16, "wd") for _ in range(4)]
        HG = alloc([128, C], F32, "hg")
        SG = alloc([128, C], F32, "sg")
        HL = alloc([128, C], F32, "hl")
        gsl = alloc([128, 4], F32, "gsl")
        wcnt = [0]
        dcnt = [0]
        scnt = [0]

        def issue_wu(ex, q):
            Wq = wu[wcnt[0] % 2]
            DMA("pool", Wq.ap, d_wup[ex, q], (), [Wq.b], "d_wu%d" % (wcnt[0] % 2))
            wcnt[0] += 1
            return Wq

        PT2 = [PT, alloc([128, 4, 512], BF16, "PTb")]
        gsl2 = [gsl, alloc([128, 4], F32, "gslb")]

        def onehot(ex):
            for j in range(16):
                TS("dve", Pm.ap[:, j, :], iotaC.ap, RK.ap[:, j, ex:ex + 1], G.ap[:, j, ex:ex + 1],
                   ALU.is_equal, ALU.mult, [iotaC.b, RK.b, G.b], [Pm.b])
            TS("dve", Pm.ap, Pm.ap, 0.0, None, ALU.is_gt, None, [Pm.b], [Pm.b])

        def gather(ex):
            for dc in range(8):
                pb = dc % 2
                for sbk in range(4):
                    for i2 in range(4):
                        j = sbk * 4 + i2
                        MM(PS[pb][:, sbk * 128:(sbk + 1) * 128], h1b.ap[:, j, dc * 128:(dc + 1) * 128], Pm.ap[:, j, :],
                           i2 == 0, i2 == 3, [h1b.b, Pm.b], [PB[pb]])
                CP("act", XeT.ap[:, dc, :], PS[pb][:, :], [PB[pb]], [XeT.b])

        def trans(ex):
            PTe = PT2[ex % 2]
            gse = gsl2[ex % 2]
            for s2 in range(2):
                pb = 2 + s2
                ptp = PS[pb][:, :].bitcast(BF16).rearrange("p (a t) -> p a t", a=8)
                for a in range(8):
                    j = s2 * 8 + a
                    TR(ptp[:, a, :], Pm.ap[:, j, :], ident_b.ap, [Pm.b, ident_b.b], [PB[pb]])
                CP("act", PTe.ap[:, s2 * 2:s2 * 2 + 2, :].rearrange("p a t -> p (a t)"), ptp.rearrange("p a t -> p (a t)"),
                   [PB[pb]], [PTe.b])
            pgs = PS[7][:, 0:8].rearrange("p (r two) -> p r two", r=4)
            for sbk in range(4):
                for i2 in range(4):
                    j = sbk * 4 + i2
                    MM(pgs[:, sbk, :], Pm.ap[:, j, :], Ghl.ap[:, j, ex, :], i2 == 0, i2 == 3, [Pm.b, Ghl.b], [PB[7]])
            S.add("dve", lambda e: e.tensor_reduce(out=gse.ap, in_=pgs, axis=AX.X, op=ALU.add), [PB[7]], [gse.b])

        def up(ex):
            wds = []
            for qd in range(4):
                Wd = wd[dcnt[0] % 4]
                DMA("pool", Wd.ap, d_wdn[ex, qd // 2][:, (qd % 2) * 2:(qd % 2) * 2 + 2, :], (), [Wd.b], "d_wd%d" % (dcnt[0] % 4))
                dcnt[0] += 1
                wds.append(Wd)
            for q in range(4):
                Wq = issue_wu(ex, q)
                for c in range(2):
                    i2 = scnt[0] % 2
                    scnt[0] += 1
                    pg = 2 * i2
                    pl = 2 * i2 + 1
                    for k in range(8):
                        MM(PS[pg][:, :], Wq.ap[:, k, c * 128:(c + 1) * 128], XeT.ap[:, k, :], k == 0, k == 7,
                           [Wq.b, XeT.b], [PB[pg]])
                    for k in range(8):
                        MM(PS[pl][:, :], Wq.ap[:, k, (2 + c) * 128:(3 + c) * 128], XeT.ap[:, k, :], k == 0, k == 7,
                           [Wq.b, XeT.b], [PB[pl]])
                    TS("dve", HG.ap, PS[pg][:, :], bup.ap[:, ex, q * 4 + c:q * 4 + c + 1], 7.0, ALU.add, ALU.min,
                       [PB[pg], bup.b], [HG.b])
                    ACT(SG.ap, HG.ap, AF.Silu, [HG.b], [SG.b], scale=1.702)
                    TS("dve", HL.ap, PS[pl][:, :], bup.ap[:, ex, q * 4 + 2 + c:q * 4 + 3 + c], 8.0, ALU.add, ALU.min,
                       [PB[pl], bup.b], [HL.b])
                    STT("dve", aT.ap[:, 2 * q + c, :], HL.ap, -6.0, SG.ap, ALU.max, ALU.mult, [HL.b, SG.b], [aT.b])
            return wds

        def down(ex, wds):
            gse = gsl2[ex % 2]
            for rc in range(4):
                for hf in range(2):
                    pb = 4 + (rc % 2) * 2 + hf
                    for fc in range(8):
                        MM(PS[pb][:, :], aT.ap[:, fc, rc * 128:(rc + 1) * 128],
                           wds[fc // 2].ap[:, fc % 2, hf * 512:(hf + 1) * 512], fc == 0, fc == 7,
                           [aT.b, wds[fc // 2].b], [PB[pb]])
                    ACT(Ye.ap[:, rc, hf * 512:(hf + 1) * 512], PS[pb][:, :], AF.Copy, [PB[pb], gse.b], [Ye.b],
                        scale=gse.ap[:, rc:rc + 1])

        def scatter(ex):
            PTe = PT2[ex % 2]
            for j in range(16):
                sbk = j // 4
                for hf in range(2):
                    pb = 4 + ((j * 2 + hf) % 4)
                    MM(PS[pb][:, :], PTe.ap[:, sbk, (j % 4) * 128:(j % 4 + 1) * 128], Ye.ap[:, sbk, hf * 512:(hf + 1) * 512],
                       True, True, [PTe.b, Ye.b], [PB[pb]])
                    TT("dve", acc.ap[:, j, hf * 512:(hf + 1) * 512], acc.ap[:, j, hf * 512:(hf + 1) * 512], PS[pb][:, :],
                       ALU.add, [acc.b, PB[pb]], [acc.b])

        onehot(0)
        gather(0)
        trans(0)
        for ex in range(32):
            if ex + 1 < 32:
                onehot(ex + 1)
            wds = up(ex)
            if ex + 1 < 32:
                gather(ex + 1)
                trans(ex + 1)
            down(ex, wds)
            scatter(ex)
        S.barrier()
        top[0] = p5_keep
        lnp = alloc([128, 2, 1024], F32, "lnp")
        for i in range(2):
            DMA("sp", lnp.ap[:, i, :], d_ln[2 + i:3 + i, :].partition_broadcast(128), (), [lnp.b], "d_ln")
        for blk in range(16):
            S.add("act", lambda e: e.memzero(sm.ap), [sm.b], [sm.b])
            layernorm(acc.ap[:, blk, :], [acc.b], acc.ap[:, blk, :], [acc.b], 0, 1)
            DMA("sp", out_d[blk], acc.ap[:, blk, :], [acc.b], (), "d_out")

        S.emit(es, final_dma_sems=["d_out"])
    return nc


def _tile_k(w):
    k, n = w.shape
    return np.ascontiguousarray(w.reshape(k // 128, 128, n).transpose(1, 0, 2))


_PROG = {}


def _host_tables(p):
    H = 8
    gam = 1.0 - 2.0 ** (-5.0 - np.arange(H, dtype=np.float64))
    lg = np.log(gam)
    i = np.arange(128, dtype=np.float64)
    i_abs = i + 128 * p
    dk = 128.0 ** -0.5
    tab = np.zeros((128, 2, H, 128), np.float64)
    for X in range(2):
        j_abs = i + 128 * X
        rel = i_abs[None, :] - j_abs[:, None]
        for h in range(H):
            tab[:, X, h, :] = np.where(rel >= 0, np.exp(np.maximum(rel, 0) * lg[h]), 0.0) * dk
    qw = np.zeros((128, H, 128), np.float64)
    for h in range(H):
        qw[:, h, :] = np.exp((i_abs + 1.0) * lg[h])[None, :]
    kw = np.zeros((128, 2, H), np.float64)
    for X in range(2):
        for h in range(H):
            kw[:, X, h] = np.exp((255.0 - (i + 128 * X)) * lg[h]) * dk
    cd2 = np.broadcast_to(np.exp(256.0 * lg)[None, :], (128, H))
    causal = (i[None, :] >= i[:, None]).astype(np.float64)
    mask = np.zeros((128, 2, 128), np.float64)
    if p == 0:
        mask[:, 0, :] = causal
        mask[:, 1, :] = 0.0
    else:
        mask[:, 0, :] = 1.0
        mask[:, 1, :] = causal
    f32 = lambda a: np.ascontiguousarray(a, dtype=np.float32)
    return {"tab": f32(tab), "qwt": f32(qw), "kwt": f32(kw), "cd2": f32(cd2), "mask": f32(mask)}


def kernel(x, positions, w_in, q_norm_g, w_uq, kv_norm_g, w_ukv, w_o, ln1_g, ln1_b,
           w_router, b_router, w_up, b_up, w_down, b_down, ln2_g, ln2_b):
    f = lambda a: np.asarray(a, dtype=np.float32)
    x = f(x)
    positions = np.asarray(positions, dtype=np.int32)
    w_in = f(w_in)[0]
    shared = {}
    shared["wcq"] = _tile_k(w_in[:, 0:384])
    shared["wckv"] = _tile_k(w_in[:, 384:704])
    shared["wrq"] = _tile_k(w_in[:, 704:1728])
    shared["wrk"] = _tile_k(w_in[:, 1728:2752])
    shared["wrv"] = _tile_k(w_in[:, 2752:3776])
    r_g = w_in[:, 3776:4800]
    g_mla = w_in[:, 4800:5824]
    g_ret = w_in[:, 5824:6848]
    shared["wgate"] = np.stack([
        _tile_k(np.concatenate([g_mla[:, h * 128:(h + 1) * 128], g_ret[:, h * 128:(h + 1) * 128],
                                r_g[:, h * 128:(h + 1) * 128]], axis=1)) for h in range(8)])
    wuq = f(w_uq)[0].reshape(384, 8, 192)
    shared["wuq"] = _tile_k(np.concatenate([wuq[:, :, 0:128].reshape(384, 1024), wuq[:, :, 128:192].reshape(384, 512)], axis=1))
    shared["qg"] = np.ascontiguousarray(f(q_norm_g)[0].reshape(3, 128).T)
    shared["wukv"] = _tile_k(f(w_ukv)[0])
    shared["kvg"] = np.ascontiguousarray(f(kv_norm_g)[0].reshape(2, 128).T)
    shared["wo"] = _tile_k(f(w_o)[0])
    shared["lnp"] = np.ascontiguousarray(np.stack([f(ln1_g)[0], f(ln1_b)[0], f(ln2_g)[0], f(ln2_b)[0]]))
    shared["wr"] = _tile_k(f(w_router)[0])
    shared["br"] = np.ascontiguousarray(f(b_router)[0][None, :])
    wu = f(w_up)[0]
    wu = wu.reshape(32, 8, 128, 2, 4, 256)
    wu = wu.transpose(0, 4, 2, 1, 3, 5)
    shared["wup"] = np.ascontiguousarray(wu.reshape(32, 4, 128, 8, 512))
    bu = f(b_up)[0].reshape(32, 2, 4, 2, 128)
    shared["bup"] = np.ascontiguousarray(bu.transpose(4, 0, 2, 1, 3).reshape(128, 32, 16))
    wd = f(w_down)[0].reshape(32, 2, 4, 128, 1024)
    shared["wdn"] = np.ascontiguousarray(wd.transpose(0, 1, 3, 2, 4))
    shared["bdn"] = np.ascontiguousarray(f(b_down)[0])
    inv_r = (1.0 / (np.float32(10000.0) ** np.linspace(0.0, 1.0, 64, dtype=np.float32))).astype(np.float64)
    inv_m = (1.0 / (np.float32(10000.0) ** (np.arange(0, 64, 2, dtype=np.float32) / np.float32(64)))).astype(np.float64)
    invf = np.concatenate([inv_r, inv_m]) / (2.0 * np.pi)
    shared["invf"] = np.ascontiguousarray(np.broadcast_to(invf[None, :], (128, 96)), dtype=np.float32)
    tabs = [_host_tables(0), _host_tables(1)]
    in_maps = []
    for c in range(8):
        b, p = c // 2, c % 2
        xb = x[b]
        xo = xb.reshape(32, 128, 1024)[p::2]
        m = dict(shared)
        m.update(tabs[p])
        m["xT_all"] = _tile_k(np.ascontiguousarray(xb.T))
        m["xT_own"] = _tile_k(np.ascontiguousarray(xo.reshape(2048, 1024).T))
        m["x_own"] = np.ascontiguousarray(xo)
        pb = positions[b].reshape(32, 128)
        m["pos_all"] = np.ascontiguousarray(pb.T)
        m["pos_own"] = np.ascontiguousarray(pb[p::2].T)
        in_maps.append(m)
    if "nc" not in _PROG:
        _PROG["nc"] = build_program()
    res = run_bass_kernel_spmd(_PROG["nc"], in_maps, core_ids=list(range(8)))
    out = np.zeros((4, 32, 128, 1024), np.float32)
    for c in range(8):
        b, p = c // 2, c % 2
        out[b, p::2] = res.results[c]["out"]
    return out.reshape(4, 4096, 1024)
```
